# Optimizing a Trainium2 kernel written in Bass

```python
import math
import jax, jax.numpy as jnp
from jax import lax
import numpy as np

D_MODEL = 1024
BATCH = 4
SEQ = 8192
DEPTH = 2

HEAD_DIM = 64
NSA_HEADS = 4
NSA_W = NSA_HEADS * HEAD_DIM
NSA_CMP_LEN = 32
NSA_CMP_STRIDE = 16
NSA_CMP_HIDDEN = 256
NSA_SEL_BLOCK = 64
NSA_TOPN = 16
NSA_WINDOW = 512
MOBA_HEADS = 4
MOBA_W = MOBA_HEADS * HEAD_DIM
MOBA_BLOCK = 256
MOBA_TOPK = 3
CONV_CH = 256
CONV_WIDTH = 31
HGRN_HEADS = 4
HGRN_KDIM = 64
HGRN_VDIM = 64
HGRN_W = HGRN_HEADS * HGRN_VDIM
HGRN_CHUNK = 64
MIX_WIDTH = NSA_W + MOBA_W + CONV_CH + HGRN_W
IN_WIDTHS = (NSA_W, HEAD_DIM, HEAD_DIM, HEAD_DIM, HEAD_DIM, HEAD_DIM, HEAD_DIM, 3 * NSA_HEADS,
             MOBA_W, MOBA_W, MOBA_W, 2 * CONV_CH, HGRN_W, HGRN_W, HGRN_W, HGRN_W)
IN_COLS = NSA_W + 6 * HEAD_DIM + 3 * NSA_HEADS + 3 * MOBA_W + 2 * CONV_CH + 4 * HGRN_W
D_FF = 4 * D_MODEL
REL_BUCKETS = 32
REL_MAX_EXACT = 16
REL_MAX_DIST = 128
N_ATTN_HEADS = NSA_HEADS + MOBA_HEADS
Q_BLOCK = 128
LN_EPS = 1e-5
RMS_EPS = 1e-6
BIG = 1e9
DEEPNORM_ALPHA = (2 * DEPTH) ** 0.25
DEEPNORM_BETA = (8 * DEPTH) ** -0.25

kernel_name = 'hymba_style_nsa_moba_conv_hgrn2_block'


def layer_norm(x, g, b):
    xf = x.astype(jnp.float32)
    mu = jnp.mean(xf, -1, keepdims=True)
    var = jnp.mean(jnp.square(xf - mu), -1, keepdims=True)
    return ((xf - mu) * lax.rsqrt(var + LN_EPS)).astype(x.dtype) * g + b


def masked_softmax(s, mask):
    s = jnp.where(mask, s.astype(jnp.float32), -jnp.inf)
    m = jnp.max(s, -1, keepdims=True)
    m = jnp.where(jnp.isfinite(m), m, 0.0)
    p = jnp.where(mask, jnp.exp(s - m), 0.0)
    return p / jnp.maximum(jnp.sum(p, -1, keepdims=True), 1e-30)


def rel_bucket(dist):
    n = jnp.maximum(dist, 0)
    nf = jnp.maximum(n, 1).astype(jnp.float32)
    large = REL_MAX_EXACT + (jnp.log(nf / REL_MAX_EXACT) / math.log(REL_MAX_DIST / REL_MAX_EXACT)
                             * (REL_BUCKETS - REL_MAX_EXACT)).astype(jnp.int32)
    return jnp.where(n < REL_MAX_EXACT, n, jnp.minimum(large, REL_BUCKETS - 1))


def nsa_compress(k, pe, w1, b1, w2):
    B, S, dh = k.shape
    n_cmp = (S - NSA_CMP_LEN) // NSA_CMP_STRIDE + 1
    idx = jnp.arange(n_cmp)[:, None] * NSA_CMP_STRIDE + jnp.arange(NSA_CMP_LEN)[None, :]
    blocks = (k[:, idx] + pe).reshape(B, n_cmp, NSA_CMP_LEN * dh)
    return jax.nn.gelu(blocks @ w1 + b1) @ w2


def nsa_attention(q, kc, vc, ks, vs, kw, vw, gates, tab):
    B, S, H, dh = q.shape
    n_cmp = kc.shape[1]
    n_sel = S // NSA_SEL_BLOCK
    topn = min(NSA_TOPN, n_sel)
    scale = dh ** -0.5
    cmp_start = jnp.arange(n_cmp) * NSA_CMP_STRIDE
    cmp_end = cmp_start + NSA_CMP_LEN - 1
    sel_start = jnp.arange(n_sel) * NSA_SEL_BLOCK
    overlap = jnp.clip(jnp.minimum(cmp_end[:, None] + 1, sel_start[None, :] + NSA_SEL_BLOCK)
                       - jnp.maximum(cmp_start[:, None], sel_start[None, :]), 0, None).astype(jnp.float32) / NSA_CMP_LEN
    kw_pad = jnp.pad(kw, ((0, 0), (NSA_WINDOW, 0), (0, 0)))
    vw_pad = jnp.pad(vw, ((0, 0), (NSA_WINDOW, 0), (0, 0)))
    bi = jnp.arange(B)[:, None, None]
    jsel = jnp.arange(n_sel)

    def block(c):
        s0 = c * Q_BLOCK
        qb = lax.dynamic_slice_in_dim(q, s0, Q_BLOCK, axis=1) * scale
        gb = lax.dynamic_slice_in_dim(gates, s0, Q_BLOCK, axis=1)
        t = s0 + jnp.arange(Q_BLOCK)
        dist_c = t[:, None] - cmp_end[None, :]
        s_c = jnp.einsum('bthd,bnd->bhtn', qb, kc) + tab[:, rel_bucket(dist_c)][None]
        p_c = masked_softmax(s_c, dist_c >= 0)
        o_c = jnp.einsum('bhtn,bnd->bthd', p_c.astype(vc.dtype), vc)
        imp = jnp.einsum('bhtn,nj->btj', p_c, overlap)
        blk_t = t // NSA_SEL_BLOCK
        forced = (jsel[None, :] == 0) | (jsel[None, :] == blk_t[:, None]) | (jsel[None, :] == blk_t[:, None] - 1)
        valid = jsel[None, :] <= blk_t[:, None]
        imp = jnp.where(forced, BIG, jnp.where(valid, imp, -BIG))
        _, sel = lax.top_k(imp, topn)
        pos = (sel[..., None] * NSA_SEL_BLOCK + jnp.arange(NSA_SEL_BLOCK)).reshape(B, Q_BLOCK, topn * NSA_SEL_BLOCK)
        k_sel = ks[bi, pos]
        v_sel = vs[bi, pos]
        dist_s = t[None, :, None] - pos
        s_s = jnp.einsum('bthd,btkd->bhtk', qb, k_sel) + jnp.moveaxis(tab[:, rel_bucket(dist_s)], 0, 1)
        p_s = masked_softmax(s_s, (dist_s >= 0)[:, None])
        o_s = jnp.einsum('bhtk,btkd->bthd', p_s.astype(v_sel.dtype), v_sel)
        kwb = lax.dynamic_slice_in_dim(kw_pad, s0, Q_BLOCK + NSA_WINDOW, axis=1)
        vwb = lax.dynamic_slice_in_dim(vw_pad, s0, Q_BLOCK + NSA_WINDOW, axis=1)
        kpos = s0 - NSA_WINDOW + jnp.arange(Q_BLOCK + NSA_WINDOW)
        dist_w = t[:, None] - kpos[None, :]
        mask_w = (dist_w >= 0) & (dist_w < NSA_WINDOW) & (kpos[None, :] >= 0)
        s_w = jnp.einsum('bthd,bkd->bhtk', qb, kwb) + tab[:, rel_bucket(dist_w)][None]
        p_w = masked_softmax(s_w, mask_w)
        o_w = jnp.einsum('bhtk,bkd->bthd', p_w.astype(vwb.dtype), vwb)
        return gb[..., 0:1] * o_c + gb[..., 1:2] * o_s + gb[..., 2:3] * o_w

    out = lax.map(block, jnp.arange(S // Q_BLOCK))
    return jnp.moveaxis(out, 0, 1).reshape(B, S, H * dh)


def moba_attention(q, k, v, tab):
    B, S, H, dh = q.shape
    s_pad = -(-S // MOBA_BLOCK) * MOBA_BLOCK
    nb = s_pad // MOBA_BLOCK
    ktop = min(MOBA_TOPK, nb)
    scale = dh ** -0.5
    padw = ((0, 0), (0, s_pad - S), (0, 0), (0, 0))
    kbh = jnp.pad(k, padw).reshape(B, nb, MOBA_BLOCK, H, dh).transpose(0, 3, 1, 2, 4)
    vbh = jnp.pad(v, padw).reshape(B, nb, MOBA_BLOCK, H, dh).transpose(0, 3, 1, 2, 4)
    k_mean = jnp.mean(kbh, axis=3)
    bi = jnp.arange(B)[:, None, None, None]
    hi = jnp.arange(H)[None, :, None, None]
    jblk = jnp.arange(nb)

    def block(c):
        s0 = c * Q_BLOCK
        qb = lax.dynamic_slice_in_dim(q, s0, Q_BLOCK, axis=1) * scale
        t = s0 + jnp.arange(Q_BLOCK)
        own = s0 // MOBA_BLOCK
        gate = jnp.einsum('bthd,bhnd->bhtn', qb, k_mean).astype(jnp.float32)
        gate = jnp.where((jblk < own)[None, None, None, :], gate, -BIG)
        gv, sel = lax.top_k(gate, ktop)
        sel_ok = gv > -0.5 * BIG
        k_sel = kbh[bi, hi, sel].reshape(B, H, Q_BLOCK, ktop * MOBA_BLOCK, dh)
        v_sel = vbh[bi, hi, sel].reshape(B, H, Q_BLOCK, ktop * MOBA_BLOCK, dh)
        pos_sel = (sel[..., None] * MOBA_BLOCK + jnp.arange(MOBA_BLOCK)).reshape(B, H, Q_BLOCK, ktop * MOBA_BLOCK)
        bias_sel = tab[hi, rel_bucket(t[None, None, :, None] - pos_sel)]
        s_sel = jnp.einsum('bthd,bhtkd->bhtk', qb, k_sel) + bias_sel
        mask_sel = jnp.repeat(sel_ok, MOBA_BLOCK, axis=-1)
        k_own = lax.dynamic_slice_in_dim(kbh, own, 1, axis=2)[:, :, 0]
        v_own = lax.dynamic_slice_in_dim(vbh, own, 1, axis=2)[:, :, 0]
        pos_own = own * MOBA_BLOCK + jnp.arange(MOBA_BLOCK)
        dist_own = t[:, None] - pos_own[None, :]
        s_own = jnp.einsum('bthd,bhkd->bhtk', qb, k_own) + tab[:, rel_bucket(dist_own)][None]
        mask_own = jnp.broadcast_to(dist_own >= 0, (B, H, Q_BLOCK, MOBA_BLOCK))
        p = masked_softmax(jnp.concatenate([s_sel, s_own], -1), jnp.concatenate([mask_sel, mask_own], -1))
        p_sel = p[..., :ktop * MOBA_BLOCK].astype(v.dtype)
        p_own = p[..., ktop * MOBA_BLOCK:].astype(v.dtype)
        return jnp.einsum('bhtk,bhtkd->bthd', p_sel, v_sel) + jnp.einsum('bhtk,bhkd->bthd', p_own, v_own)

    out = lax.map(block, jnp.arange(S // Q_BLOCK))
    return jnp.moveaxis(out, 0, 1).reshape(B, S, H * dh)


def conformer_conv(u, w_dw, b_dw, ln_g, ln_b, w_pw):
    a, g = jnp.split(u, 2, axis=-1)
    h = a * jax.nn.sigmoid(g)
    h = lax.conv_general_dilated(h, w_dw[:, None, :], window_strides=(1,),
                                 padding=[(CONV_WIDTH - 1, 0)],
                                 dimension_numbers=('NWC', 'WIO', 'NWC'),
                                 feature_group_count=CONV_CH) + b_dw
    h = jax.nn.silu(layer_norm(h, ln_g, ln_b))
    return h @ w_pw


def hgrn2_mixer(q, f_logit, i, g, lb, norm_g):
    B, S, _ = q.shape
    H, dk, dv, C = HGRN_HEADS, HGRN_KDIM, HGRN_VDIM, HGRN_CHUNK
    nc = S // C
    f = lb + (1.0 - lb) * jax.nn.sigmoid(f_logit.astype(jnp.float32))
    log_f = jnp.log(f)
    k = 1.0 - f

    def chunks(a, d):
        return a.astype(jnp.float32).reshape(B, nc, C, H, d).transpose(1, 0, 3, 2, 4)

    causal = jnp.tril(jnp.ones((C, C), dtype=bool))[:, :, None]

    def step(state, inp):
        qc, kc, vc, gc = inp
        cum = jnp.cumsum(gc, axis=2)
        diff = cum[:, :, :, None, :] - cum[:, :, None, :, :]
        decay = jnp.where(causal, jnp.exp(jnp.where(causal, diff, 0.0)), 0.0)
        attn = jnp.einsum('bhtk,bhsk,bhtsk->bhts', qc, kc, decay)
        o = jnp.einsum('bhtk,bhkv->bhtv', qc * jnp.exp(cum), state) + jnp.einsum('bhts,bhsv->bhtv', attn, vc)
        last = cum[:, :, -1:, :]
        state = jnp.exp(last[:, :, 0, :])[..., None] * state + jnp.einsum('bhsk,bhsv->bhkv', kc * jnp.exp(last - cum), vc)
        return state, o

    s_init = jnp.zeros((B, H, dk, dv), jnp.float32)
    _, o = lax.scan(step, s_init, (chunks(q, dk), chunks(k, dk), chunks(i, dv), chunks(log_f, dk)))
    o = o.transpose(1, 0, 3, 2, 4).reshape(B, S, H, dv)
    o = o * lax.rsqrt(jnp.mean(o * o, -1, keepdims=True) + RMS_EPS)
    return o.reshape(B, S, H * dv).astype(q.dtype) * norm_g * jax.nn.sigmoid(g)


def hybrid_layer(x, w_in, cmp_pe, cmp_w1, cmp_b1, cmp_w2, dw_w, dw_b, cln_g, cln_b, pw_w,
                 lb, hn_g, w_out, ln1_g, ln1_b, w_ff1, w_ff2, ln2_g, ln2_b, rel_tab):
    B, S, _ = x.shape
    split_points = [int(v) for v in np.cumsum(IN_WIDTHS)[:-1]]
    h = x @ w_in
    (q_n, kc, vc, ks, vs, kw, vw, g_n, q_m, k_m, v_m, conv_in,
     q_h, f_h, i_h, g_h) = jnp.split(h, split_points, axis=-1)
    kc = nsa_compress(kc, cmp_pe[0], cmp_w1[0], cmp_b1[0], cmp_w2[0])
    vc = nsa_compress(vc, cmp_pe[1], cmp_w1[1], cmp_b1[1], cmp_w2[1])
    gates = jax.nn.sigmoid(g_n.reshape(B, S, NSA_HEADS, 3))
    o_nsa = nsa_attention(q_n.reshape(B, S, NSA_HEADS, HEAD_DIM), kc, vc, ks, vs, kw, vw, gates,
                          rel_tab[:NSA_HEADS])
    o_moba = moba_attention(q_m.reshape(B, S, MOBA_HEADS, HEAD_DIM), k_m.reshape(B, S, MOBA_HEADS, HEAD_DIM),
                            v_m.reshape(B, S, MOBA_HEADS, HEAD_DIM), rel_tab[NSA_HEADS:])
    o_conv = conformer_conv(conv_in, dw_w, dw_b, cln_g, cln_b, pw_w)
    o_hgrn = hgrn2_mixer(q_h, f_h, i_h, g_h, lb, hn_g)
    mixed = jnp.concatenate([o_nsa, o_moba, o_conv, o_hgrn], axis=-1) @ w_out
    x = layer_norm(DEEPNORM_ALPHA * x + mixed, ln1_g, ln1_b)
    ff = jnp.square(jax.nn.relu(x @ w_ff1)) @ w_ff2
    return layer_norm(DEEPNORM_ALPHA * x + ff, ln2_g, ln2_b)


def setup_inputs(seed: int = 0) -> dict:
    key = jax.random.key(seed)
    ks = jax.random.split(key, 24)

    def nrm(k, shape, scale):
        return jax.random.normal(k, shape, jnp.float32) * scale

    return {
        'x': nrm(ks[0], (BATCH, SEQ, D_MODEL), 1.0),
        'w_in': nrm(ks[1], (DEPTH, D_MODEL, IN_COLS), D_MODEL ** -0.5),
        'nsa_cmp_pe': nrm(ks[2], (DEPTH, 2, NSA_CMP_LEN, HEAD_DIM), 0.1),
        'nsa_cmp_w1': nrm(ks[3], (DEPTH, 2, NSA_CMP_LEN * HEAD_DIM, NSA_CMP_HIDDEN), (NSA_CMP_LEN * HEAD_DIM) ** -0.5),
        'nsa_cmp_b1': nrm(ks[4], (DEPTH, 2, NSA_CMP_HIDDEN), 0.02),
        'nsa_cmp_w2': nrm(ks[5], (DEPTH, 2, NSA_CMP_HIDDEN, HEAD_DIM), NSA_CMP_HIDDEN ** -0.5),
        'conv_dw_w': nrm(ks[6], (DEPTH, CONV_WIDTH, CONV_CH), CONV_WIDTH ** -0.5),
        'conv_dw_b': nrm(ks[7], (DEPTH, CONV_CH), 0.02),
        'conv_ln_g': 1.0 + nrm(ks[8], (DEPTH, CONV_CH), 0.02),
        'conv_ln_b': nrm(ks[9], (DEPTH, CONV_CH), 0.02),
        'conv_pw_w': nrm(ks[10], (DEPTH, CONV_CH, CONV_CH), CONV_CH ** -0.5),
        'hgrn_lb_logits': nrm(ks[11], (DEPTH, HGRN_HEADS * HGRN_KDIM), 0.5),
        'hgrn_norm_g': 1.0 + nrm(ks[12], (DEPTH, HGRN_W), 0.02),
        'w_out': nrm(ks[13], (DEPTH, MIX_WIDTH, D_MODEL), MIX_WIDTH ** -0.5 * DEEPNORM_BETA),
        'ln1_g': 1.0 + nrm(ks[14], (DEPTH, D_MODEL), 0.02),
        'ln1_b': nrm(ks[15], (DEPTH, D_MODEL), 0.02),
        'w_ff1': nrm(ks[16], (DEPTH, D_MODEL, D_FF), D_MODEL ** -0.5),
        'w_ff2': nrm(ks[17], (DEPTH, D_FF, D_MODEL), D_FF ** -0.5 * DEEPNORM_BETA),
        'ln2_g': 1.0 + nrm(ks[18], (DEPTH, D_MODEL), 0.02),
        'ln2_b': nrm(ks[19], (DEPTH, D_MODEL), 0.02),
        'rel_bias': nrm(ks[20], (N_ATTN_HEADS, REL_BUCKETS), 0.5),
    }


def reference(x, w_in, nsa_cmp_pe, nsa_cmp_w1, nsa_cmp_b1, nsa_cmp_w2, conv_dw_w, conv_dw_b,
              conv_ln_g, conv_ln_b, conv_pw_w, hgrn_lb_logits, hgrn_norm_g, w_out, ln1_g, ln1_b,
              w_ff1, w_ff2, ln2_g, ln2_b, rel_bias):
    lb_sm = jax.nn.softmax(hgrn_lb_logits.astype(jnp.float32), axis=0)
    lbs = (jnp.cumsum(lb_sm, axis=0) - lb_sm[0]).astype(x.dtype)
    for l in range(DEPTH):
        x = hybrid_layer(x, w_in[l], nsa_cmp_pe[l], nsa_cmp_w1[l], nsa_cmp_b1[l], nsa_cmp_w2[l],
                         conv_dw_w[l], conv_dw_b[l], conv_ln_g[l], conv_ln_b[l], conv_pw_w[l],
                         lbs[l], hgrn_norm_g[l], w_out[l], ln1_g[l], ln1_b[l], w_ff1[l], w_ff2[l],
                         ln2_g[l], ln2_b[l], rel_bias)
    return x
```

```python
import math
from contextlib import ExitStack
import numpy as np
import ml_dtypes
import concourse.bass as bass
import concourse.mybir as mybir
from concourse.bass_utils import run_bass_kernel_spmd

F32 = mybir.dt.float32
BF16 = mybir.dt.bfloat16
AF = mybir.ActivationFunctionType
ALU = mybir.AluOpType
AX = mybir.AxisListType
NPBF = ml_dtypes.bfloat16

D_MODEL = 1024
BATCH = 4
SEQ = 8192
DEPTH = 2
D_FF = 4096
ALPHA = (2 * DEPTH) ** 0.25
LN_EPS = 1e-5
RMS_EPS = 1e-6
NEG = -30000.0


def _act_copy(nc, out, in_):
    return nc.scalar.activation(out=out, in_=in_, func=AF.Copy)


class Tok:
    __slots__ = ("w", "r")

    def __init__(self):
        self.w = {}
        self.r = {}


class Ctx:
    def __init__(self, nc, stack, ndma=24):
        self.nc = nc
        self.eng = {"pe": nc.tensor, "act": nc.scalar, "dve": nc.vector, "pool": nc.gpsimd, "sp": nc.sync}
        self.sems = {}
        self.cnt = {}
        for n in ["pe", "act", "dve", "pool"]:
            self.sems[n] = stack.enter_context(nc.semaphore("s_" + n))
            self.cnt[n] = 0
        self.ndma = ndma
        for i in range(ndma):
            n = "d%d" % i
            self.sems[n] = stack.enter_context(nc.semaphore("s_" + n))
            self.cnt[n] = 0
        self.rr = 0
        self.seen = {e: {} for e in self.eng}

    def _wait(self, e, s, v):
        if v <= 0 or self.seen[e].get(s, 0) >= v:
            return
        self.eng[e].wait_ge(self.sems[s], v)
        self.seen[e][s] = v

    def _deps(self, e, reads, writes):
        need = {}
        for t in reads:
            for s, v in t.w.items():
                if need.get(s, 0) < v:
                    need[s] = v
        for t in writes:
            for s, v in t.w.items():
                if need.get(s, 0) < v:
                    need[s] = v
            for s, v in t.r.items():
                if need.get(s, 0) < v:
                    need[s] = v
        if e == "pe":
            need.pop("pe", None)
        for s, v in need.items():
            self._wait(e, s, v)

    def op(self, e, fn, reads=(), writes=()):
        self._deps(e, reads, writes)
        ins = fn()
        self.cnt[e] += 1
        c = self.cnt[e]
        ins.then_inc(self.sems[e], 1)
        for t in reads:
            t.r[e] = c
        for t in writes:
            t.w = {e: c}
            t.r = {}
        return ins

    def dma(self, e, out, in_, reads=(), writes=()):
        s = "d%d" % self.rr
        self.rr = (self.rr + 1) % self.ndma
        self._wait(e, s, self.cnt[s])
        self._deps(e, reads, writes)
        ins = self.eng[e].dma_start(out=out, in_=in_)
        self.cnt[s] += 16
        c = self.cnt[s]
        ins.then_inc(self.sems[s], 16)
        for t in reads:
            t.r[s] = c
        for t in writes:
            t.w[s] = c
            t.r = {}
        return ins

    def barrier(self):
        for e in self.eng:
            for s, v in self.cnt.items():
                if s != e:
                    self._wait(e, s, v)

    def finish(self):
        for s, v in self.cnt.items():
            self._wait("sp", s, v)


def layer_norm_tile(C, nc, y, yt, out_ap, out_tok, G, Bv, ctok, st, sttok, tmp, tmptok):
    for c in range(2):
        C.op("dve", lambda c=c: nc.vector.bn_stats(out=st[:, c * 6:(c + 1) * 6], in_=y[:, c * 512:(c + 1) * 512]),
             reads=[yt], writes=[sttok])
    C.op("dve", lambda: nc.vector.bn_aggr(out=st[:, 12:14], in_=st[:, 0:12]), reads=[sttok], writes=[sttok])
    C.op("dve", lambda: nc.vector.tensor_scalar(out=st[:, 14:15], in0=st[:, 13:14], scalar1=LN_EPS, scalar2=None,
                                                op0=ALU.add), reads=[sttok], writes=[sttok])
    C.op("act", lambda: nc.scalar.activation(out=st[:, 15:16], in_=st[:, 14:15], func=AF.Sqrt),
         reads=[sttok], writes=[sttok])
    C.op("dve", lambda: nc.vector.reciprocal(out=st[:, 16:17], in_=st[:, 15:16]), reads=[sttok], writes=[sttok])
    C.op("dve", lambda: nc.vector.tensor_scalar(out=tmp, in0=y, scalar1=st[:, 12:13], scalar2=st[:, 16:17],
                                                op0=ALU.subtract, op1=ALU.mult),
         reads=[yt, sttok], writes=[tmptok])
    C.op("pool", lambda: nc.gpsimd.tensor_tensor(out=tmp, in0=tmp, in1=G, op=ALU.mult),
         reads=[tmptok, ctok], writes=[tmptok])
    C.op("pool", lambda: nc.gpsimd.tensor_tensor(out=out_ap, in0=tmp, in1=Bv, op=ALU.add),
         reads=[tmptok, ctok], writes=[out_tok])


def load_cast(C, nc, dst, dst_tok, src_dram, stage, stage_toks, k, rows=128, eng_cycle=("dve", "pool", "act")):
    sb = stage[k % len(stage)]
    st = stage_toks[k % len(stage)]
    n = src_dram.shape[-1]
    C.dma("sp", sb[:rows, :n], src_dram, writes=[st])
    e = eng_cycle[k % len(eng_cycle)]
    if e == "act":
        C.op("act", lambda: nc.scalar.copy(out=dst, in_=sb[:rows, :n]), reads=[st], writes=[dst_tok])
    elif e == "dve":
        C.op("dve", lambda: nc.vector.tensor_copy(out=dst, in_=sb[:rows, :n]), reads=[st], writes=[dst_tok])
    else:
        C.op("pool", lambda: nc.gpsimd.tensor_copy(out=dst, in_=sb[:rows, :n]), reads=[st], writes=[dst_tok])


def _transpose_tile(C, nc, ident, wt, ptr, ptr_t, src_bf, src_tok, dst_ap, dst_tok, kk):
    p = ptr[kk % 2]
    pt = ptr_t[kk % 2]
    for fc in range(8):
        C.op("pe", lambda fc=fc: nc.tensor.transpose(out=p[:, fc, :], in_=src_bf[:, fc * 128:(fc + 1) * 128],
                                                    identity=ident[:, :]),
             reads=[src_tok, wt], writes=[pt])
    C.op("act", lambda: nc.scalar.copy(out=dst_ap, in_=p[:, :, :]), reads=[pt], writes=[dst_tok])


def emit_tail_a(C, nc, ntok, xres, catT, w_out, lnc, x1d, x1Td, ident_d):
    GT = 512
    ng = ntok // GT
    with ExitStack() as es:
        def sb(name, shape, dt):
            return es.enter_context(nc.sbuf_tensor(name, shape, dt))

        def ps(name, shape, dt):
            return es.enter_context(nc.psum_tensor(name, shape, dt))

        Wo = sb("Wo", [128, 8, 1024], BF16)
        LNC = sb("LNCa", [128, 2, 1024], F32)
        ident = sb("identa", [128, 128], BF16)
        stage = [sb("stga%d" % i, [128, 1024], F32) for i in range(2)]
        stage_t = [Tok(), Tok()]
        wt = Tok()
        C.dma("sp", ident[:, :], ident_d, writes=[wt])
        for i in range(2):
            C.dma("sp", LNC[:, i, :], lnc[i], writes=[wt])
        for fc in range(8):
            load_cast(C, nc, Wo[:, fc, :], wt, w_out[fc * 128:(fc + 1) * 128, :], stage, stage_t, fc)
        cT = [sb("cT%d" % i, [128, 8, GT], BF16) for i in range(2)]
        cT_t = [Tok() for _ in range(2)]
        xr = [sb("xra%d" % i, [128, 1024], F32) for i in range(3)]
        xr_t = [Tok() for _ in range(3)]
        xo = [sb("xoa%d" % i, [128, 1024], F32) for i in range(2)]
        xo_t = [Tok() for _ in range(2)]
        xb = [sb("xba%d" % i, [128, 1024], BF16) for i in range(2)]
        xb_t = [Tok() for _ in range(2)]
        xT = [sb("xTa%d" % i, [128, 8, 128], BF16) for i in range(2)]
        xT_t = [Tok() for _ in range(2)]
        st = [sb("sta%d" % i, [128, 32], F32) for i in range(2)]
        st_t = [Tok() for _ in range(2)]
        pm = [ps("pma%d" % i, [128, 1024], F32) for i in range(2)]
        pm_t = [Tok() for _ in range(2)]
        ptr = [ps("ptra%d" % i, [128, 8, 128], BF16) for i in range(2)]
        ptr_t = [Tok() for _ in range(2)]
        kk = 0
        for g in range(ng):
            c = cT[g % 2]
            ct = cT_t[g % 2]
            C.dma("sp", c[:, :, :], catT[:, g * GT:(g + 1) * GT].rearrange("(fc p) t -> p fc t", p=128), writes=[ct])
            for tt in range(GT // 128):
                r0 = g * GT + tt * 128
                p = pm[kk % 2]; pt = pm_t[kk % 2]
                for n in range(2):
                    for fc in range(8):
                        C.op("pe", lambda n=n, fc=fc: nc.tensor.matmul(
                            p[:, n * 512:(n + 1) * 512], lhsT=c[:, fc, tt * 128:(tt + 1) * 128],
                            rhs=Wo[:, fc, n * 512:(n + 1) * 512], start=(fc == 0), stop=(fc == 7)),
                            reads=[ct, wt], writes=[pt])
                x_ = xr[kk % 3]; x_t = xr_t[kk % 3]
                C.dma("sp", x_[:, :], xres[r0:r0 + 128, :], writes=[x_t])
                C.op("dve", lambda: nc.vector.scalar_tensor_tensor(out=x_[:, :], in0=x_[:, :], scalar=ALPHA, in1=p[:, :],
                                                                   op0=ALU.mult, op1=ALU.add),
                     reads=[x_t, pt], writes=[x_t])
                o_ = xo[kk % 2]; o_t = xo_t[kk % 2]
                layer_norm_tile(C, nc, x_[:, :], x_t, o_[:, :], o_t, LNC[:, 0, :], LNC[:, 1, :], wt,
                                st[kk % 2], st_t[kk % 2], x_[:, :], x_t)
                C.dma("pool", x1d[r0:r0 + 128, :], o_[:, :], reads=[o_t])
                b_ = xb[kk % 2]; bt = xb_t[kk % 2]
                C.op("dve", lambda: nc.vector.tensor_copy(out=b_[:, :], in_=o_[:, :]), reads=[o_t], writes=[bt])
                _transpose_tile(C, nc, ident, wt, ptr, ptr_t, b_, bt, xT[kk % 2][:, :, :], xT_t[kk % 2], kk)
                C.dma("pool", x1Td[:, r0:r0 + 128].rearrange("(fc p) t -> p fc t", p=128), xT[kk % 2][:, :, :],
                      reads=[xT_t[kk % 2]])
                kk += 1
        C.barrier()


def emit_tail_b(C, nc, ntok, x1d, x1Td, w_ff1, w_ff2, lnc, xnext, xnextT, ident_d):
    GT = 256
    ng = ntok // GT
    with ExitStack() as es:
        def sb(name, shape, dt):
            return es.enter_context(nc.sbuf_tensor(name, shape, dt))

        def ps(name, shape, dt):
            return es.enter_context(nc.psum_tensor(name, shape, dt))

        W1 = sb("W1", [128, 8, 4096], BF16)
        W2 = sb("W2", [128, 32, 1024], BF16)
        LNC = sb("LNCb", [128, 2, 1024], F32)
        ident = sb("identb", [128, 128], BF16)
        wt = Tok()
        C.dma("sp", ident[:, :], ident_d, writes=[wt])
        for i in range(2):
            C.dma("sp", LNC[:, i, :], lnc[2 + i], writes=[wt])
        with nc.sbuf_tensor("stgb0", [128, 1024], F32) as s0, nc.sbuf_tensor("stgb1", [128, 1024], F32) as s1:
            stage = [s0, s1]
            stage_t = [Tok(), Tok()]
            k = 0
            for fc in range(8):
                for h in range(4):
                    load_cast(C, nc, W1[:, fc, h * 1024:(h + 1) * 1024], wt,
                              w_ff1[fc * 128:(fc + 1) * 128, h * 1024:(h + 1) * 1024], stage, stage_t, k); k += 1
            for fc in range(32):
                load_cast(C, nc, W2[:, fc, :], wt, w_ff2[fc * 128:(fc + 1) * 128, :], stage, stage_t, k); k += 1
            C.barrier()
        xT = [sb("xTb%d" % i, [128, 8, GT], BF16) for i in range(2)]
        xT_t = [Tok() for _ in range(2)]
        h1T = sb("h1T", [128, 32, GT], BF16)
        h1T_t = [Tok() for _ in range(32)]
        sq = [sb("sq%d" % i, [128, GT], F32) for i in range(2)]
        sq_t = [Tok() for _ in range(2)]
        xr = [sb("xrb%d" % i, [128, 1024], F32) for i in range(2)]
        xr_t = [Tok() for _ in range(2)]
        ob = [sb("ob%d" % i, [128, 1024], F32) for i in range(2)]
        ob_t = [Tok() for _ in range(2)]
        xb = [sb("xbb%d" % i, [128, 1024], BF16) for i in range(2)]
        xb_t = [Tok() for _ in range(2)]
        oT = [sb("oTb%d" % i, [128, 8, 128], BF16) for i in range(2)]
        oT_t = [Tok() for _ in range(2)]
        st = [sb("stb%d" % i, [128, 32], F32) for i in range(2)]
        st_t = [Tok() for _ in range(2)]
        pm = [ps("pmb%d" % i, [128, 1024], F32) for i in range(2)]
        pm_t = [Tok() for _ in range(2)]
        ph = [ps("phb%d" % i, [128, 512], F32) for i in range(2)]
        ph_t = [Tok() for _ in range(2)]
        ptr = [ps("ptrb%d" % i, [128, 8, 128], BF16) for i in range(2)]
        ptr_t = [Tok() for _ in range(2)]
        kk = 0
        for g in range(ng):
            x_T = xT[g % 2]; x_Tt = xT_t[g % 2]
            C.dma("sp", x_T[:, :, :], x1Td[:, g * GT:(g + 1) * GT].rearrange("(fc p) t -> p fc t", p=128), writes=[x_Tt])
            for ffc in range(32):
                p = ph[ffc % 2]; pt = ph_t[ffc % 2]
                for fc in range(8):
                    C.op("pe", lambda fc=fc, ffc=ffc: nc.tensor.matmul(
                        p[:, 0:GT], lhsT=W1[:, fc, ffc * 128:(ffc + 1) * 128], rhs=x_T[:, fc, :],
                        start=(fc == 0), stop=(fc == 7)), reads=[x_Tt, wt], writes=[pt])
                s_ = sq[ffc % 2]; s_t = sq_t[ffc % 2]
                C.op("act", lambda: nc.scalar.activation(out=s_[:, :], in_=p[:, 0:GT], func=AF.Square),
                     reads=[pt], writes=[s_t])
                C.op("dve", lambda ffc=ffc: nc.vector.scalar_tensor_tensor(
                    out=h1T[:, ffc, :], in0=p[:, 0:GT], scalar=0.0, in1=s_[:, :], op0=ALU.is_gt, op1=ALU.mult),
                    reads=[pt, s_t], writes=[h1T_t[ffc]])
            for tt in range(GT // 128):
                r0 = g * GT + tt * 128
                p = pm[kk % 2]; pt = pm_t[kk % 2]
                for n in range(2):
                    for ffc in range(32):
                        C.op("pe", lambda n=n, ffc=ffc: nc.tensor.matmul(
                            p[:, n * 512:(n + 1) * 512], lhsT=h1T[:, ffc, tt * 128:(tt + 1) * 128],
                            rhs=W2[:, ffc, n * 512:(n + 1) * 512], start=(ffc == 0), stop=(ffc == 31)),
                            reads=[h1T_t[ffc], wt], writes=[pt])
                x_ = xr[kk % 2]; x_t = xr_t[kk % 2]
                C.dma("sp", x_[:, :], x1d[r0:r0 + 128, :], writes=[x_t])
                C.op("dve", lambda: nc.vector.scalar_tensor_tensor(out=x_[:, :], in0=x_[:, :], scalar=ALPHA, in1=p[:, :],
                                                                   op0=ALU.mult, op1=ALU.add),
                     reads=[x_t, pt], writes=[x_t])
                o_ = ob[kk % 2]; o_t = ob_t[kk % 2]
                layer_norm_tile(C, nc, x_[:, :], x_t, o_[:, :], o_t, LNC[:, 0, :], LNC[:, 1, :], wt,
                                st[kk % 2], st_t[kk % 2], x_[:, :], x_t)
                C.dma("pool", xnext[r0:r0 + 128, :], o_[:, :], reads=[o_t])
                if xnextT is not None:
                    b_ = xb[kk % 2]; bt = xb_t[kk % 2]
                    C.op("dve", lambda: nc.vector.tensor_copy(out=b_[:, :], in_=o_[:, :]), reads=[o_t], writes=[bt])
                    _transpose_tile(C, nc, ident, wt, ptr, ptr_t, b_, bt, oT[kk % 2][:, :, :], oT_t[kk % 2], kk)
                    C.dma("pool", xnextT[:, r0:r0 + 128].rearrange("(fc p) t -> p fc t", p=128), oT[kk % 2][:, :, :],
                          reads=[oT_t[kk % 2]])
                kk += 1
        C.barrier()


FM_BLOCKS = [("QN0", 128, "q"), ("QN1", 128, "q"), ("KVC", 128, ""), ("KSW", 128, ""), ("GN", 6, "sig"),
             ("MQ", 128, "q"), ("MK", 128, ""), ("CV0", 128, ""), ("CV1", 128, ""), ("CV2", 128, ""), ("CV3", 128, ""),
             ("HQ", 128, ""), ("HF", 128, ""), ("HG", 128, "sig")]
FM_OFF = {}
_o = 0
for _n, _c, _k in FM_BLOCKS:
    FM_OFF[_n] = _o
    _o += 128
NFM_ROWS = _o
NFM_COLS = sum(c for _, c, _ in FM_BLOCKS)
NTM_COLS = 512


def emit_proj(C, nc, S, xT, x_f32, wfm, wtm, FMS, TMS, HFT):
    CH = 512
    nch = S // CH
    with ExitStack() as es:
        def sb(name, shape, dt):
            return es.enter_context(nc.sbuf_tensor(name, shape, dt))

        def ps(name, shape, dt):
            return es.enter_context(nc.psum_tensor(name, shape, dt))
        Wf = sb("Wf", [128, 8, NFM_COLS], BF16)
        Wt = sb("Wt", [128, 8, NTM_COLS], BF16)
        wtok = Tok()
        stage = [sb("pstg%d" % i, [128, 2048], F32) for i in range(2)]
        stage_t = [Tok(), Tok()]
        k = 0
        for fc in range(8):
            load_cast(C, nc, Wf[:, fc, :], wtok, wfm[fc * 128:(fc + 1) * 128, :], stage, stage_t, k); k += 1
            load_cast(C, nc, Wt[:, fc, :], wtok, wtm[fc * 128:(fc + 1) * 128, :], stage, stage_t, k); k += 1
        xs = [sb("pxs%d" % i, [128, 8, CH], F32) for i in range(2)] if x_f32 else None
        xs_t = [Tok(), Tok()]
        xb = [sb("pxb%d" % i, [128, 8, CH], BF16) for i in range(2)]
        xb_t = [Tok(), Tok()]
        ob = [sb("pob%d" % i, [128, CH], BF16) for i in range(4)]
        ob_t = [Tok() for _ in range(4)]
        of = [sb("pof%d" % i, [128, 128], F32) for i in range(2)]
        of_t = [Tok() for _ in range(2)]
        pp = [ps("ppp%d" % i, [128, CH], F32) for i in range(4)]
        pp_t = [Tok() for _ in range(4)]
        ke = 0
        for ch in range(nch):
            xbt = xb[ch % 2]; xbtt = xb_t[ch % 2]
            src = xT[:, ch * CH:(ch + 1) * CH].rearrange("(fc p) t -> p fc t", p=128)
            if x_f32:
                C.dma("sp", xs[ch % 2][:, :, :], src, writes=[xs_t[ch % 2]])
                if ch % 2 == 0:
                    C.op("dve", lambda: nc.vector.tensor_copy(out=xbt[:, :, :], in_=xs[ch % 2][:, :, :]),
                         reads=[xs_t[ch % 2]], writes=[xbtt])
                else:
                    C.op("pool", lambda: nc.gpsimd.tensor_copy(out=xbt[:, :, :], in_=xs[ch % 2][:, :, :]),
                         reads=[xs_t[ch % 2]], writes=[xbtt])
            else:
                C.dma("sp", xbt[:, :, :], src, writes=[xbtt])
            c0 = 0
            import os
            PROBE = os.environ.get("PROBE", "")
            for (name, ncol, kind) in FM_BLOCKS:
                if PROBE == "4":
                    break
                if PROBE == "1" and name == "GN":
                    c0 += ncol
                    continue
                if PROBE == "2" and kind == "q":
                    kind = ""
                p = pp[ke % 4]; pt = pp_t[ke % 4]
                for fc in range(8):
                    C.op("pe", lambda fc=fc, c0=c0, ncol=ncol: nc.tensor.matmul(
                        p[:ncol, :], lhsT=Wf[:, fc, c0:c0 + ncol], rhs=xbt[:, fc, :], start=(fc == 0), stop=(fc == 7)),
                        reads=[wtok, xbtt], writes=[pt])
                o = ob[ke % 4]; ot = ob_t[ke % 4]
                if kind == "sig":
                    C.op("act", lambda: nc.scalar.activation(out=o[:ncol, :], in_=p[:ncol, :], func=AF.Sigmoid),
                         reads=[pt], writes=[ot])
                elif kind == "q":
                    C.op("act", lambda: nc.scalar.activation(out=o[:ncol, :], in_=p[:ncol, :], func=AF.Copy, scale=0.125),
                         reads=[pt], writes=[ot])
                else:
                    C.op("dve", lambda: nc.vector.tensor_copy(out=o[:ncol, :], in_=p[:ncol, :]), reads=[pt], writes=[ot])
                r0 = FM_OFF[name]
                C.dma("pool", FMS[r0:r0 + ncol, ch * CH:(ch + 1) * CH], o[:ncol, :], reads=[ot])
                c0 += ncol
                ke += 1
            for tt in range(CH // 128):
                if PROBE == "3":
                    break
                p = pp[ke % 4]; pt = pp_t[ke % 4]
                for fc in range(8):
                    C.op("pe", lambda fc=fc, tt=tt: nc.tensor.matmul(
                        p[:, :], lhsT=xbt[:, fc, tt * 128:(tt + 1) * 128], rhs=Wt[:, fc, :], start=(fc == 0), stop=(fc == 7)),
                        reads=[wtok, xbtt], writes=[pt])
                o = ob[ke % 4]; ot = ob_t[ke % 4]
                C.op("dve", lambda: nc.vector.tensor_copy(out=o[:, 0:384], in_=p[:, 0:384]), reads=[pt], writes=[ot])
                f_ = of[ke % 2]; ft = of_t[ke % 2]
                t0 = ch * CH + tt * 128
                if PROBE != "5":
                    C.op("dve", lambda: nc.vector.tensor_copy(out=f_[:, :], in_=p[:, 384:512]), reads=[pt], writes=[ft])
                    C.dma("pool", HFT[t0:t0 + 128, :], f_[:, :], reads=[ft])
                if PROBE != "6":
                    C.dma("pool", TMS[t0:t0 + 128, :], o[:, 0:384], reads=[ot])
                ke += 1
        C.barrier()


class Rot:
    def __init__(self, bufs):
        self.b = bufs
        self.t = [Tok() for _ in bufs]
        self.i = 0

    def next(self):
        k = self.i % len(self.b)
        self.i += 1
        return self.b[k], self.t[k]


def attn_chunk(C, nc, A, qc, QT, qtok, KT, ktok, V1, vtok, kts, bias_of, dyn, post_exp, consumer):
    ot, ott = A["OT"].next()
    n = len(kts)
    for i, kt in enumerate(kts):
        s, st = A["S"].next()
        b = bias_of(kt)
        C.op("pe", lambda: nc.tensor.matmul(s[:, :], lhsT=KT[:, kt * 128:(kt + 1) * 128], rhs=QT[:, qc * 512:(qc + 1) * 512],
                                            start=True, stop=(b is None and dyn is None)),
             reads=[qtok, ktok], writes=[st])
        if b is not None:
            C.op("pe", lambda: nc.tensor.matmul(s[:, :], lhsT=A["ident"][:, :], rhs=b[0], start=False, stop=(dyn is None)),
                 reads=[b[1], A["ctok"]], writes=[st])
        if dyn is not None:
            C.op("pe", lambda: nc.tensor.matmul(s[:, :], lhsT=dyn[0][:, kt * 128:(kt + 1) * 128], rhs=dyn[2],
                                                start=False, stop=True),
                 reads=[dyn[1], dyn[3]], writes=[st])
        p, pt = A["P"].next()
        C.op("act", lambda: nc.scalar.activation(out=p[:, :], in_=s[:, :], func=AF.Exp), reads=[st], writes=[pt])
        C.op("pe", lambda: nc.tensor.matmul(ot[:65, :], lhsT=V1[:, kt, :], rhs=p[:, :], start=(i == 0), stop=(i == n - 1)),
             reads=[vtok, pt], writes=[ott])
        if post_exp is not None:
            post_exp(kt, i, n, p, pt)
    consumer(ot, ott)


def emit_nsa(C, nc, S, FMS, TMS, cw1, cb1, cw2, cpe, BTc, BTs, BTw, t31, STAT, OV, Gd, cst, OUT):
    NQ = S // 512
    NSEL = S // 64
    NCMP = (S - 32) // 16 + 1
    NTC = (NCMP + 127) // 128
    NKT = S // 128
    with ExitStack() as es:
        def sb(name, shape, dt):
            return es.enter_context(nc.sbuf_tensor(name, shape, dt))

        def ps(name, shape, dt):
            return es.enter_context(nc.psum_tensor(name, shape, dt))
        ctok = Tok()
        ident = sb("n_ident", [128, 128], BF16)
        onesf = sb("n_onesf", [128, 64], F32)
        m127 = sb("n_m127", [128, 1], F32)
        C.dma("sp", ident[:, :], cst["ident"], writes=[ctok])
        C.dma("sp", onesf[:, :], cst["ones64"], writes=[ctok])
        C.dma("sp", m127[:, :], cst["m127"], writes=[ctok])
        A = {"ident": ident, "ctok": ctok,
             "S": Rot([ps("n_S%d" % i, [128, 512], F32) for i in range(3)]),
             "OT": Rot([ps("n_OT%d" % i, [128, 512], F32) for i in range(2)]),
             "P": Rot([sb("n_P%d" % i, [128, 512], BF16) for i in range(4)])}
        pU = Rot([ps("n_U%d" % i, [128, 4, 128], F32) for i in range(1)])
        pX = ps("n_X", [128, 512], F32)
        pXt = Tok()
        QN = sb("n_QN", [128, 2, S], BF16)
        qtok = Tok()
        for b in range(2):
            r0 = FM_OFF["QN%d" % b]
            C.dma("sp", QN[:, b, :], FMS[r0:r0 + 128, :], writes=[qtok])
        KS2 = sb("n_KS2", [128, S], BF16); KW2 = sb("n_KW2", [128, S], BF16)
        kstok = Tok(); kwtok = Tok()
        for hh in range(2):
            C.dma("sp", KS2[hh * 64:(hh + 1) * 64, :], FMS[FM_OFF["KSW"]:FM_OFF["KSW"] + 64, :], writes=[kstok])
            C.dma("sp", KW2[hh * 64:(hh + 1) * 64, :], FMS[FM_OFF["KSW"] + 64:FM_OFF["KSW"] + 128, :], writes=[kwtok])
        VS1 = sb("n_VS1", [128, NKT, 65], BF16); VW1 = sb("n_VW1", [128, NKT, 65], BF16)
        vstok = Tok(); vwtok = Tok()
        C.dma("sp", VS1[:, :, 0:64], TMS[:, 0:64].rearrange("(kt p) d -> p kt d", p=128), writes=[vstok])
        C.dma("sp", VW1[:, :, 0:64], TMS[:, 64:128].rearrange("(kt p) d -> p kt d", p=128), writes=[vwtok])
        C.op("dve", lambda: nc.vector.memset(VS1[:, :, 64:65], 1.0), writes=[vstok])
        vstok.w.update({k_: v_ for k_, v_ in [("dve", C.cnt["dve"])]})
        C.op("dve", lambda: nc.vector.memset(VW1[:, :, 64:65], 1.0), writes=[vwtok])
        C.barrier()
        G = sb("n_G", [128, S], BF16)
        gtok = Tok()
        C.dma("sp", G[:NSEL, :], Gd, writes=[gtok])
        OVs = sb("n_OV", [128, NTC, NSEL], BF16)
        ovtok = Tok()
        T31 = sb("n_t31", [128, 4], F32)
        Bc = sb("n_Bc", [128, 20, 512], BF16); Bs = sb("n_Bs", [128, 10, 512], BF16); Bw = sb("n_Bw", [128, 16, 512], BF16)
        KC2 = sb("n_KC2", [128, NTC * 128], BF16)
        VC1 = sb("n_VC1", [128, NTC, 65], BF16)
        es2 = ExitStack()

        def sb2(name, shape, dt):
            return es2.enter_context(nc.sbuf_tensor(name, shape, dt))
        stg = sb2("n_stg", [128, 2048], F32)
        stgt = Tok()
        for nt in range(NTC):
            C.dma("sp", stg[:, :NSEL], OV[nt * 128:(nt + 1) * 128, :], writes=[stgt])
            C.op("dve", lambda nt=nt: nc.vector.tensor_copy(out=OVs[:, nt, :], in_=stg[:, :NSEL]), reads=[stgt], writes=[ovtok])
        C.dma("sp", T31[:, :], t31, writes=[ctok])
        btok = Tok()
        for (dst, srcd, nper) in ((Bc, BTc, 5), (Bs, BTs, 5), (Bw, BTw, 8)):
            for i in range(srcd.shape[0]):
                C.dma("sp", stg[:, :512], srcd[i], writes=[stgt])
                h = i // nper
                C.op("dve", lambda dst=dst, i=i, h=h: nc.vector.tensor_scalar(
                    out=dst[:, i, :], in0=stg[:, :512], scalar1=T31[:, h:h + 1], scalar2=None, op0=ALU.subtract),
                    reads=[stgt, ctok], writes=[btok])
        kctok = Tok(); vctok = Tok()
        XR = sb2("n_XR", [64, S], BF16)
        W1c = sb2("n_W1c", [64, 32, 256], BF16)
        W2c = sb2("n_W2c", [128, 2, 64], BF16)
        peT = sb2("n_peT", [64, 32], BF16)
        b1c = sb2("n_b1c", [128, 2], F32)
        bcol = sb2("n_bcol", [128, 2], F32)
        GT_ = sb2("n_GT", [128, 2, NTC * 128], BF16)
        gx = sb2("n_gx", [128, NTC * 128], F32); gu = sb2("n_gu", [128, NTC * 128], F32)
        xrt = Tok(); w1t = Tok(); gtt = Tok(); gxt = Tok()
        C.op("dve", lambda: nc.vector.memset(GT_[:, :, :], 0.0), writes=[gtt])
        C.op("dve", lambda: nc.vector.memset(VC1[:, :, :], 0.0), writes=[vctok])
        C.op("dve", lambda: nc.vector.memset(KC2[:, :], 0.0), writes=[kctok])
        for which in range(2):
            r0 = FM_OFF["KVC"] + 64 * which
            C.dma("sp", XR[:, :], FMS[r0:r0 + 64, :], writes=[xrt])
            for i4 in range(8):
                C.dma("sp", stg[:64, :1024].rearrange("d (i h) -> d i h", h=256),
                      cw1[which, i4 * 256:(i4 + 1) * 256, :].rearrange("(i d) h -> d i h", d=64), writes=[stgt])
                C.op("dve", lambda i4=i4: nc.vector.tensor_copy(
                    out=W1c[:, i4 * 4:(i4 + 1) * 4, :], in_=stg[:64, :1024].rearrange("d (i h) -> d i h", h=256)),
                    reads=[stgt], writes=[w1t])
            C.dma("sp", stg[:, :128].rearrange("p (c d) -> p c d", d=64), cw2[which].rearrange("(c p) d -> p c d", p=128), writes=[stgt])
            C.op("dve", lambda: nc.vector.tensor_copy(out=W2c[:, :, :], in_=stg[:, :128].rearrange("p (c d) -> p c d", d=64)),
                 reads=[stgt], writes=[w1t])
            C.dma("sp", stg[:64, :32], cpe[which].rearrange("i d -> d i"), writes=[stgt])
            C.op("dve", lambda: nc.vector.tensor_copy(out=peT[:, :], in_=stg[:64, :32]), reads=[stgt], writes=[w1t])
            C.dma("sp", b1c[:, :], cb1[which].rearrange("(c p) -> p c", p=128), writes=[w1t])
            for hc in range(2):
                for i in range(32):
                    C.op("pe", lambda i=i, hc=hc: nc.tensor.matmul(pX[:, 0:1], lhsT=W1c[:, i, hc * 128:(hc + 1) * 128],
                                                                  rhs=peT[:, i:i + 1], start=(i == 0), stop=(i == 31)),
                         reads=[w1t], writes=[pXt])
                C.op("dve", lambda hc=hc: nc.vector.tensor_tensor(out=bcol[:, hc:hc + 1], in0=pX[:, 0:1], in1=b1c[:, hc:hc + 1], op=ALU.add),
                     reads=[pXt, w1t], writes=[gxt])
                for i in range(32):
                    C.op("pe", lambda i=i, hc=hc: nc.tensor.matmul(
                        pX[:, 0:NCMP], lhsT=W1c[:, i, hc * 128:(hc + 1) * 128], rhs=XR[:, i:i + 16 * (NCMP - 1) + 1:16],
                        start=(i == 0), stop=(i == 31)), reads=[w1t, xrt], writes=[pXt])
                C.op("dve", lambda hc=hc: nc.vector.tensor_scalar(out=gx[:, :NCMP], in0=pX[:, 0:NCMP], scalar1=bcol[:, hc:hc + 1],
                                                                  scalar2=None, op0=ALU.add), reads=[pXt, gxt], writes=[gxt])
                C.op("dve", lambda: nc.vector.tensor_tensor(out=gu[:, :NCMP], in0=gx[:, :NCMP], in1=gx[:, :NCMP], op=ALU.mult),
                     reads=[gxt], writes=[gxt])
                C.op("dve", lambda: nc.vector.tensor_scalar(out=gu[:, :NCMP], in0=gu[:, :NCMP], scalar1=0.044715, scalar2=1.0,
                                                            op0=ALU.mult, op1=ALU.add), reads=[gxt], writes=[gxt])
                C.op("dve", lambda: nc.vector.tensor_tensor(out=gu[:, :NCMP], in0=gu[:, :NCMP], in1=gx[:, :NCMP], op=ALU.mult),
                     reads=[gxt], writes=[gxt])
                C.op("act", lambda: nc.scalar.activation(out=gu[:, :NCMP], in_=gu[:, :NCMP], func=AF.Sigmoid,
                                                         scale=2.0 * math.sqrt(2.0 / math.pi)), reads=[gxt], writes=[gxt])
                C.op("dve", lambda hc=hc: nc.vector.tensor_tensor(out=GT_[:, hc, :NCMP], in0=gu[:, :NCMP], in1=gx[:, :NCMP], op=ALU.mult),
                     reads=[gxt], writes=[gtt])
            if which == 0:
                for hc in range(2):
                    C.op("pe", lambda hc=hc: nc.tensor.matmul(pX[:64, 0:NTC * 128], lhsT=W2c[:, hc, :], rhs=GT_[:, hc, :],
                                                              start=(hc == 0), stop=(hc == 1)), reads=[w1t, gtt], writes=[pXt])
                C.op("dve", lambda: nc.vector.tensor_copy(out=KC2[0:64, :], in_=pX[:64, 0:NTC * 128]), reads=[pXt], writes=[kctok])
                C.dma("sp", KC2[64:128, :], KC2[0:64, :], reads=[kctok], writes=[kctok])
            else:
                for nt in range(NTC):
                    for hc in range(2):
                        C.op("pe", lambda hc=hc, nt=nt: nc.tensor.matmul(pX[:, 0:64], lhsT=GT_[:, hc, nt * 128:(nt + 1) * 128],
                                                                        rhs=W2c[:, hc, :], start=(hc == 0), stop=(hc == 1)),
                             reads=[w1t, gtt], writes=[pXt])
                    C.op("dve", lambda nt=nt: nc.vector.tensor_copy(out=VC1[:, nt, 0:64], in_=pX[:, 0:64]), reads=[pXt], writes=[vctok])
                C.op("dve", lambda: nc.vector.memset(VC1[:, :, 64:65], 1.0), reads=[], writes=[vctok])
                lastn = NCMP - (NTC - 1) * 128
                assert lastn == 127
                C.op("dve", lambda: nc.vector.tensor_scalar(out=VC1[:, NTC - 1, :], in0=VC1[:, NTC - 1, :], scalar1=m127[:, 0:1],
                                                            scalar2=None, op0=ALU.mult), reads=[ctok], writes=[vctok])
        C.barrier()
        es2.close()
        NM = Rot([sb("n_NM%d" % i, [128, 512], BF16) for i in range(2)])
        impacc = sb("n_imp", [128, 4, NSEL], F32); imptok = Tok()
        stat = Rot([sb("n_stat%d" % i, [128, 4, NSEL], F32) for i in range(2)])
        wk = sb("n_wk", [128, NSEL], F32); m8 = sb("n_m8", [128, 16], F32); rr = sb("n_rr", [128, 4], F32)
        nmq = sb("n_nmq", [128, 128], BF16); wkt = Tok()
        if NSEL < 128:
            C.op("dve", lambda: nc.vector.memset(nmq[:, :], 0.0), writes=[wkt])
        pTf = ps("n_pT", [128, 1024], BF16); pT = pTf[:, 0:128]; pTt = Tok()
        gq = Rot([sb("n_gq%d" % i, [128, 6, 512], BF16) for i in range(2)])
        OTs = Rot([sb("n_OTs%d" % i, [128, 512], F32) for i in range(2)])
        fac = Rot([sb("n_fac%d" % i, [128, 512], F32) for i in range(2)])
        acc = [sb("n_acc%d" % i, [64, 512], F32) for i in range(2)]
        acct = [Tok(), Tok()]
        tmpm = Rot([sb("n_tm%d" % i, [64, 512], F32) for i in range(2)])
        oo = Rot([sb("n_oo%d" % i, [64, 512], BF16) for i in range(2)])
        for qc in range(NQ):
            g_, g_t = gq.next()
            C.dma("sp", g_[64:65, :, :], FMS[FM_OFF["GN"]:FM_OFF["GN"] + 6, qc * 512:(qc + 1) * 512].rearrange("(o r) t -> o r t", o=1),
                  writes=[g_t])
            stt_, stt_t = stat.next()
            C.dma("sp", stt_[:, :, :], STAT[qc * 4:(qc + 1) * 4].rearrange("q p j -> p q j"), writes=[stt_t])
            first_branch = [True, True]

            def make_consumer(hl, grow):
                def consumer(ot, ott):
                    o_s, o_st = OTs.next()
                    C.op("act", lambda: _act_copy(nc, out=o_s[:65, :], in_=ot[:65, :]), reads=[ott], writes=[o_st])
                    f_, f_t = fac.next()
                    C.op("dve", lambda: nc.vector.tensor_scalar(out=f_[64:65, :], in0=o_s[64:65, :], scalar1=1e-30, scalar2=None,
                                                                op0=ALU.max), reads=[o_st], writes=[f_t])
                    C.op("dve", lambda: nc.vector.reciprocal(out=f_[64:65, :], in_=f_[64:65, :]), reads=[f_t], writes=[f_t])
                    C.op("dve", lambda: nc.vector.tensor_tensor(out=f_[64:65, :], in0=f_[64:65, :], in1=g_[64:65, grow, :], op=ALU.mult),
                         reads=[f_t, g_t], writes=[f_t])
                    C.op("pe", lambda: nc.tensor.matmul(pX[:64, :], lhsT=onesf[64:65, 0:64], rhs=f_[64:65, :], start=True, stop=True),
                         reads=[f_t, ctok], writes=[pXt])
                    if first_branch[hl]:
                        C.op("dve", lambda: nc.vector.tensor_tensor(out=acc[hl][:, :], in0=o_s[0:64, :], in1=pX[:64, :], op=ALU.mult),
                             reads=[o_st, pXt], writes=[acct[hl]])
                        first_branch[hl] = False
                    else:
                        t_, t_t = tmpm.next()
                        C.op("dve", lambda: nc.vector.tensor_tensor(out=t_[:, :], in0=o_s[0:64, :], in1=pX[:64, :], op=ALU.mult),
                             reads=[o_st, pXt], writes=[t_t])
                        C.op("pool", lambda: nc.gpsimd.tensor_tensor(out=acc[hl][:, :], in0=acc[hl][:, :], in1=t_[:, :], op=ALU.add),
                             reads=[t_t, acct[hl]], writes=[acct[hl]])
                return consumer

            q0 = qc * 512
            ckts = [nt for nt in range(NTC) if (q0 - 2048 * nt) > -512]
            for h in range(4):
                blk, hh = h // 2, h % 2
                U, Ut = pU.next()

                def bias_c(nt, h=h):
                    d = q0 - 2048 * nt
                    if d >= 2560:
                        return None
                    return (Bc[:, h * 5 + d // 512, :], btok)

                def post(nt, i, n, p, pt, U=U, Ut=Ut):
                    for qs in range(4):
                        C.op("pe", lambda qs=qs: nc.tensor.matmul(U[:, qs, :NSEL], lhsT=p[:, qs * 128:(qs + 1) * 128], rhs=OVs[:, nt, :],
                                                                  start=(i == 0), stop=(i == n - 1)), reads=[pt, ovtok], writes=[Ut])
                if len(ckts) == 0:
                    continue
                cons = make_consumer(h, h * 3 + 0) if h < 2 else (lambda ot, ott: None)
                attn_chunk(C, nc, A, qc, QN[hh * 64:(hh + 1) * 64, blk, :], qtok, KC2[hh * 64:(hh + 1) * 64, :], kctok, VC1, vctok,
                           ckts, bias_c, None, post, cons)
                C.op("dve", lambda U=U: nc.vector.tensor_scalar(out=rr[:, :], in0=U[:, :, NSEL - 1], scalar1=1e-30, scalar2=None, op0=ALU.max),
                     reads=[Ut], writes=[wkt])
                C.op("dve", lambda: nc.vector.reciprocal(out=rr[:, :], in_=rr[:, :]), reads=[wkt], writes=[wkt])
                for qs in range(4):
                    if h == 0:
                        C.op("dve", lambda qs=qs, U=U: nc.vector.tensor_scalar(out=impacc[:, qs, :], in0=U[:, qs, :NSEL], scalar1=rr[:, qs:qs + 1],
                                                                              scalar2=None, op0=ALU.mult), reads=[Ut, wkt], writes=[imptok])
                    else:
                        C.op("dve", lambda qs=qs, U=U: nc.vector.scalar_tensor_tensor(
                            out=impacc[:, qs, :], in0=U[:, qs, :NSEL], scalar=rr[:, qs:qs + 1], in1=impacc[:, qs, :],
                            op0=ALU.mult, op1=ALU.add), reads=[Ut, wkt, imptok], writes=[imptok])
            nm, nmt = NM.next()
            for qs in range(4):
                if len(ckts) == 0:
                    C.op("dve", lambda qs=qs: nc.vector.tensor_copy(out=impacc[:, qs, :], in_=stt_[:, qs, :]), reads=[stt_t], writes=[imptok])
                else:
                    C.op("dve", lambda qs=qs: nc.vector.tensor_tensor(out=impacc[:, qs, :], in0=impacc[:, qs, :], in1=stt_[:, qs, :], op=ALU.add),
                         reads=[stt_t, imptok], writes=[imptok])
                C.op("dve", lambda qs=qs: nc.vector.max(out=m8[:, 0:8], in_=impacc[:, qs, :]), reads=[imptok], writes=[wkt])
                C.op("dve", lambda qs=qs: nc.vector.match_replace(out=wk[:, :], in_to_replace=m8[:, 0:8], in_values=impacc[:, qs, :],
                                                                  imm_value=-3.0e9), reads=[imptok, wkt], writes=[wkt])
                C.op("dve", lambda: nc.vector.max(out=m8[:, 8:16], in_=wk[:, :]), reads=[wkt], writes=[wkt])
                C.op("dve", lambda qs=qs: nc.vector.tensor_scalar(out=wk[:, :], in0=impacc[:, qs, :], scalar1=m8[:, 15:16], scalar2=-NEG,
                                                                  op0=ALU.is_ge, op1=ALU.mult), reads=[imptok, wkt], writes=[wkt])
                C.op("dve", lambda: nc.vector.tensor_scalar(out=nmq[:, :NSEL], in0=wk[:, :], scalar1=NEG, scalar2=None, op0=ALU.add),
                     reads=[wkt], writes=[wkt])
                C.op("pe", lambda: nc.tensor.transpose(out=pT[:, :], in_=nmq[:, :], identity=ident[:, :]), reads=[wkt, ctok], writes=[pTt])
                C.op("act", lambda qs=qs: _act_copy(nc, out=nm[:, qs * 128:(qs + 1) * 128], in_=pT[:, :]), reads=[pTt], writes=[nmt])
            for hl in range(2):
                skts = list(range(0, 4 * qc + 4))

                def bias_s(kt, hl=hl):
                    d = q0 - 128 * kt
                    if d >= 256:
                        return None
                    return (Bs[:, hl * 5 + (d + 384) // 128, :], btok)
                attn_chunk(C, nc, A, qc, QN[hl * 64:(hl + 1) * 64, 0, :], qtok, KS2[hl * 64:(hl + 1) * 64, :], kstok, VS1, vstok,
                           skts, bias_s, (G[:NSEL, :], gtok, nm[:NSEL, :], nmt), None, make_consumer(hl, hl * 3 + 1))
                wkts = list(range(max(0, 4 * qc - 4), 4 * qc + 4))

                def bias_w(kt, hl=hl):
                    d = q0 - 128 * kt
                    return (Bw[:, hl * 8 + (d + 384) // 128, :], btok)
                attn_chunk(C, nc, A, qc, QN[hl * 64:(hl + 1) * 64, 0, :], qtok, KW2[hl * 64:(hl + 1) * 64, :], kwtok, VW1, vwtok,
                           wkts, bias_w, None, None, make_consumer(hl, hl * 3 + 2))
                o_, o_t = oo.next()
                C.op("act", lambda hl=hl: _act_copy(nc, out=o_[:, :], in_=acc[hl][:, :]), reads=[acct[hl]], writes=[o_t])
                C.dma("pool", OUT[hl * 64:(hl + 1) * 64, qc * 512:(qc + 1) * 512], o_[:, :], reads=[o_t])
        C.barrier()


def emit_moba(C, nc, S, FMS, TMS, BTm, t31m, STATM, G2d, cst, OUT):
    NQ = S // 512
    NB = S // 256
    NKT = S // 128
    with ExitStack() as es:
        def sb(name, shape, dt):
            return es.enter_context(nc.sbuf_tensor(name, shape, dt))

        def ps(name, shape, dt):
            return es.enter_context(nc.psum_tensor(name, shape, dt))
        ctok = Tok()
        ident = sb("m_ident", [128, 128], BF16)
        onesf = sb("m_onesf", [128, 64], F32)
        C.dma("sp", ident[:, :], cst["ident"], writes=[ctok])
        C.dma("sp", onesf[:, :], cst["ones64"], writes=[ctok])
        A = {"ident": ident, "ctok": ctok,
             "S": Rot([ps("m_S%d" % i, [128, 512], F32) for i in range(3)]),
             "OT": Rot([ps("m_OT%d" % i, [128, 512], F32) for i in range(2)]),
             "P": Rot([sb("m_P%d" % i, [128, 512], BF16) for i in range(4)])}
        pX = ps("m_X", [128, 512], F32); pXt = Tok()
        pG = ps("m_G", [128, 512], F32); pGt = Tok()
        pTf = ps("m_pT", [128, 1024], BF16); pT = pTf[:, 0:128]; pTt = Tok()
        MQ = sb("m_MQ", [128, S], BF16); MK = sb("m_MK", [128, S], BF16)
        qtok = Tok(); ktok = Tok()
        C.dma("sp", MQ[:, :], FMS[FM_OFF["MQ"]:FM_OFF["MQ"] + 128, :], writes=[qtok])
        C.dma("sp", MK[:, :], FMS[FM_OFF["MK"]:FM_OFF["MK"] + 128, :], writes=[ktok])
        MV1 = [sb("m_MV%d" % h, [128, NKT, 65], BF16) for h in range(2)]
        vtok = [Tok(), Tok()]
        for h in range(2):
            C.dma("sp", MV1[h][:, :, 0:64], TMS[:, 128 + 64 * h:192 + 64 * h].rearrange("(kt p) d -> p kt d", p=128), writes=[vtok[h]])
        C.barrier()
        for h in range(2):
            C.op("dve", lambda h=h: nc.vector.memset(MV1[h][:, :, 64:65], 1.0), writes=[vtok[h]])
        G2 = sb("m_G2", [128, S], BF16); gtok = Tok()
        C.dma("sp", G2[:, :], G2d, writes=[gtok])
        REP = 128 // NB
        T31 = sb("m_t31", [128, 2], F32)
        C.dma("sp", T31[:, :], t31m, writes=[ctok])
        Bm = sb("m_Bm", [128, 10, 512], BF16); btok = Tok()
        stg = sb("m_stg", [128, 512], F32); stgt = Tok()
        for i in range(10):
            C.dma("sp", stg[:, :], BTm[i], writes=[stgt])
            C.op("dve", lambda i=i: nc.vector.tensor_scalar(out=Bm[:, i, :], in0=stg[:, :], scalar1=T31[:, i // 5:i // 5 + 1],
                                                            scalar2=None, op0=ALU.subtract), reads=[stgt, ctok], writes=[btok])
        kmf = sb("m_kmf", [128, 32], F32); km = sb("m_km", [128, 32], BF16); kmt = Tok()
        C.op("dve", lambda: nc.vector.tensor_reduce(out=kmf[:, :NB], in_=MK[:, :].rearrange("p (b k) -> p b k", k=256), axis=AX.X, op=ALU.add),
             reads=[ktok], writes=[kmt])
        C.op("dve", lambda: nc.vector.tensor_scalar(out=km[:, :NB], in0=kmf[:, :NB], scalar1=1.0 / 256.0, scalar2=None, op0=ALU.mult),
             reads=[kmt], writes=[kmt])
        NMm = [Rot([sb("m_NM%d_%d" % (h, i), [128, 512], BF16) for i in range(2)]) for h in range(2)]
        statm = Rot([sb("m_stat%d" % i, [128, 4, NB], F32) for i in range(2)])
        gw = sb("m_gw", [128, 32], F32); m8 = sb("m_m8", [128, 8], F32); th = sb("m_th", [128, 1], F32)
        nmq = sb("m_nmq", [128, 128], BF16); wkt = Tok()
        C.op("dve", lambda: nc.vector.memset(nmq[:, :], 0.0), writes=[wkt])
        OTs = Rot([sb("m_OTs%d" % i, [128, 512], F32) for i in range(2)])
        fac = Rot([sb("m_fac%d" % i, [128, 512], F32) for i in range(2)])
        oo = Rot([sb("m_oo%d" % i, [64, 512], BF16) for i in range(2)])
        for qc in range(NQ):
            q0 = qc * 512
            st_, st_t = statm.next()
            C.dma("sp", st_[:, :, :], STATM[qc * 4:(qc + 1) * 4].rearrange("q p j -> p q j"), writes=[st_t])
            for h in range(2):
                nm, nmt = NMm[h].next()
                for qs in range(4):
                    C.op("pe", lambda qs=qs, h=h: nc.tensor.matmul(pG[:, :NB], lhsT=MQ[h * 64:(h + 1) * 64, q0 + qs * 128:q0 + (qs + 1) * 128],
                                                                  rhs=km[h * 64:(h + 1) * 64, :NB], start=True, stop=True),
                         reads=[qtok, kmt], writes=[pGt])
                    C.op("dve", lambda qs=qs: nc.vector.tensor_tensor(out=gw[:, :NB], in0=pG[:, :NB], in1=st_[:, qs, :], op=ALU.add),
                         reads=[pGt, st_t], writes=[wkt])
                    C.op("dve", lambda: nc.vector.max(out=m8[:, :], in_=gw[:, :NB]), reads=[wkt], writes=[wkt])
                    C.op("dve", lambda: nc.vector.tensor_scalar(out=th[:, :], in0=m8[:, 3:4], scalar1=-1.0e9, scalar2=None, op0=ALU.max),
                         reads=[wkt], writes=[wkt])
                    C.op("dve", lambda: nc.vector.tensor_scalar(out=gw[:, :NB], in0=gw[:, :NB], scalar1=th[:, 0:1], scalar2=-NEG,
                                                                op0=ALU.is_ge, op1=ALU.mult), reads=[wkt], writes=[wkt])
                    for rp in range(REP):
                        C.op("dve", lambda rp=rp: nc.vector.tensor_scalar(out=nmq[:, rp * NB:(rp + 1) * NB], in0=gw[:, :NB], scalar1=NEG, scalar2=None,
                                                                        op0=ALU.add), reads=[wkt], writes=[wkt])
                    C.op("pe", lambda: nc.tensor.transpose(out=pT[:, :], in_=nmq[:, :], identity=ident[:, :]), reads=[wkt, ctok], writes=[pTt])
                    C.op("act", lambda qs=qs: _act_copy(nc, out=nm[:, qs * 128:(qs + 1) * 128], in_=pT[:, :]), reads=[pTt], writes=[nmt])
                kts = list(range(0, 4 * qc + 4))

                def bias_m(kt, h=h):
                    d = q0 - 128 * kt
                    if d >= 256:
                        return None
                    return (Bm[:, h * 5 + (d + 384) // 128, :], btok)

                def consumer(ot, ott, h=h):
                    o_s, o_st = OTs.next()
                    C.op("act", lambda: _act_copy(nc, out=o_s[:65, :], in_=ot[:65, :]), reads=[ott], writes=[o_st])
                    f_, f_t = fac.next()
                    C.op("dve", lambda: nc.vector.reciprocal(out=f_[64:65, :], in_=o_s[64:65, :]), reads=[o_st], writes=[f_t])
                    C.op("pe", lambda: nc.tensor.matmul(pX[:64, :], lhsT=onesf[64:65, 0:64], rhs=f_[64:65, :], start=True, stop=True),
                         reads=[f_t, ctok], writes=[pXt])
                    o_, o_t = oo.next()
                    C.op("dve", lambda: nc.vector.tensor_tensor(out=o_[:, :], in0=o_s[0:64, :], in1=pX[:64, :], op=ALU.mult),
                         reads=[o_st, pXt], writes=[o_t])
                    C.dma("pool", OUT[h * 64:(h + 1) * 64, q0:q0 + 512], o_[:, :], reads=[o_t])
                attn_chunk(C, nc, A, qc, MQ[h * 64:(h + 1) * 64, :], qtok, MK[h * 64:(h + 1) * 64, :], ktok, MV1[h], vtok[h],
                           kts, bias_m, (G2[:, :], gtok, nm[:, :], nmt), None, consumer)
        C.barrier()


def emit_conv(C, nc, S, FMS, dww, dwb, clg, clb, pww, cst, OUT):
    CH = 512
    nch = S // CH
    with ExitStack() as es:
        def sb(name, shape, dt):
            return es.enter_context(nc.sbuf_tensor(name, shape, dt))

        def ps(name, shape, dt):
            return es.enter_context(nc.psum_tensor(name, shape, dt))
        ctok = Tok()
        ident = sb("c_ident", [128, 128], BF16)
        C.dma("sp", ident[:, :], cst["ident"], writes=[ctok])
        onesb = sb("c_ones", [128, 128], BF16)
        C.op("dve", lambda: nc.vector.memset(onesb[:, :], 1.0), writes=[ctok])
        ctok.w["d%d" % ((C.rr - 1) % C.ndma)] = C.cnt["d%d" % ((C.rr - 1) % C.ndma)]
        wcol = sb("c_wcol", [128, 2, 31], F32)
        vec = sb("c_vec", [128, 2, 3], F32)
        for c in range(2):
            C.dma("sp", wcol[:, c, :], dww[:, c * 128:(c + 1) * 128].rearrange("j p -> p j"), writes=[ctok])
        C.dma("sp", vec[:, :, 0], dwb.rearrange("(c p) -> p c", p=128), writes=[ctok])
        C.dma("sp", vec[:, :, 1], clg.rearrange("(c p) -> p c", p=128), writes=[ctok])
        C.dma("sp", vec[:, :, 2], clb.rearrange("(c p) -> p c", p=128), writes=[ctok])
        C.barrier()
        DG = sb("c_DG", [128, 2, 31, 128], BF16)
        for c in range(2):
            for j in range(31):
                C.op("dve", lambda c=c, j=j: nc.vector.tensor_scalar(out=DG[:, c, j, :], in0=ident[:, :], scalar1=wcol[:, c, j:j + 1],
                                                                    scalar2=None, op0=ALU.mult), reads=[ctok], writes=[ctok])
        Wp = sb("c_Wp", [128, 2, 256], BF16)
        stg = sb("c_stg", [128, 256], F32); stgt = Tok()
        for c in range(2):
            C.dma("sp", stg[:, :], pww[c * 128:(c + 1) * 128, :], writes=[stgt])
            C.op("dve", lambda c=c: nc.vector.tensor_copy(out=Wp[:, c, :], in_=stg[:, :]), reads=[stgt], writes=[ctok])
        H = sb("c_H", [128, 2, 32 + S], BF16); htok = Tok()
        C.op("dve", lambda: nc.vector.memset(H[:, :, 0:32], 0.0), writes=[htok])
        a_ = Rot([sb("c_a%d" % i, [128, 2048], BF16) for i in range(2)])
        g_ = Rot([sb("c_g%d" % i, [128, 2048], BF16) for i in range(2)])
        hts = []
        for c in range(2):
            for s0 in range(0, S, 2048):
                a, at = a_.next(); g, gt = g_.next()
                C.dma("sp", a[:, :], FMS[FM_OFF["CV%d" % c]:FM_OFF["CV%d" % c] + 128, s0:s0 + 2048], writes=[at])
                C.dma("sp", g[:, :], FMS[FM_OFF["CV%d" % (2 + c)]:FM_OFF["CV%d" % (2 + c)] + 128, s0:s0 + 2048], writes=[gt])
                C.op("act", lambda: nc.scalar.activation(out=g[:, :], in_=g[:, :], func=AF.Sigmoid), reads=[gt], writes=[gt])
                ht = Tok(); hts.append(ht)
                C.op("dve", lambda c=c, s0=s0: nc.vector.tensor_tensor(out=H[:, c, 32 + s0:32 + s0 + 2048], in0=a[:, :], in1=g[:, :], op=ALU.mult),
                     reads=[at, gt, htok], writes=[ht])
        C.barrier()
        pc = Rot([ps("c_pc%d" % i, [128, 512], F32) for i in range(4)])
        pst = Rot([ps("c_pst%d" % i, [128, 512], F32) for i in range(2)])
        po = Rot([ps("c_po%d" % i, [128, 512], F32) for i in range(2)])
        hc = Rot([sb("c_hc%d" % i, [128, 2, 512], F32) for i in range(2)])
        hb = Rot([sb("c_hb%d" % i, [128, 2, 512], BF16) for i in range(2)])
        sqb = Rot([sb("c_sq%d" % i, [128, 2, 512], BF16) for i in range(2)])
        mu = Rot([sb("c_mu%d" % i, [128, 512], F32) for i in range(2)])
        rs = Rot([sb("c_rs%d" % i, [128, 512], F32) for i in range(2)])
        yn = Rot([sb("c_yn%d" % i, [128, 2, 512], F32) for i in range(2)])
        ys = Rot([sb("c_ys%d" % i, [128, 2, 512], BF16) for i in range(2)])
        ob = Rot([sb("c_ob%d" % i, [128, 512], BF16) for i in range(4)])
        for ch in range(nch):
            t0 = ch * CH
            h_, h_t = hc.next(); hb_, hb_t = hb.next(); sq_, sq_t = sqb.next()
            for c in range(2):
                p, pt = pc.next()
                for j in range(31):
                    C.op("pe", lambda c=c, j=j: nc.tensor.matmul(p[:, :], lhsT=DG[:, c, j, :], rhs=H[:, c, t0 + 2 + j:t0 + 2 + j + CH],
                                                                start=(j == 0), stop=(j == 30)), reads=[ctok], writes=[pt])
                C.op("act", lambda c=c: nc.scalar.activation(out=h_[:, c, :], in_=p[:, :], func=AF.Identity, bias=vec[:, c, 0:1]),
                     reads=[pt, ctok], writes=[h_t])
            C.op("dve", lambda: nc.vector.tensor_copy(out=hb_[:, :, :], in_=h_[:, :, :]), reads=[h_t], writes=[hb_t])
            C.op("pool", lambda: nc.gpsimd.tensor_tensor(out=sq_[:, :, :], in0=h_[:, :, :], in1=h_[:, :, :], op=ALU.mult), reads=[h_t], writes=[sq_t])
            p1, p1t = pst.next()
            for c in range(2):
                C.op("pe", lambda c=c: nc.tensor.matmul(p1[:, :], lhsT=onesb[:, :], rhs=hb_[:, c, :], start=(c == 0), stop=(c == 1)),
                     reads=[hb_t, ctok], writes=[p1t])
            p2, p2t = pst.next()
            for c in range(2):
                C.op("pe", lambda c=c: nc.tensor.matmul(p2[:, :], lhsT=onesb[:, :], rhs=sq_[:, c, :], start=(c == 0), stop=(c == 1)),
                     reads=[sq_t, ctok], writes=[p2t])
            m_, m_t = mu.next(); r_, r_t = rs.next()
            C.op("act", lambda: nc.scalar.activation(out=m_[:, :], in_=p1[:, :], func=AF.Copy, scale=1.0 / 256.0), reads=[p1t], writes=[m_t])
            C.op("dve", lambda: nc.vector.tensor_tensor(out=r_[:, :], in0=m_[:, :], in1=m_[:, :], op=ALU.mult), reads=[m_t], writes=[r_t])
            C.op("dve", lambda: nc.vector.scalar_tensor_tensor(out=r_[:, :], in0=p2[:, :], scalar=1.0 / 256.0, in1=r_[:, :],
                                                               op0=ALU.mult, op1=ALU.subtract), reads=[p2t, r_t], writes=[r_t])
            C.op("dve", lambda: nc.vector.tensor_scalar(out=r_[:, :], in0=r_[:, :], scalar1=LN_EPS, scalar2=None, op0=ALU.add),
                 reads=[r_t], writes=[r_t])
            C.op("act", lambda: nc.scalar.activation(out=r_[:, :], in_=r_[:, :], func=AF.Sqrt), reads=[r_t], writes=[r_t])
            C.op("dve", lambda: nc.vector.reciprocal(out=r_[:, :], in_=r_[:, :]), reads=[r_t], writes=[r_t])
            y_, y_t = yn.next(); s_, s_t = ys.next()
            for c in range(2):
                C.op("dve", lambda c=c: nc.vector.tensor_tensor(out=y_[:, c, :], in0=h_[:, c, :], in1=m_[:, :], op=ALU.subtract),
                     reads=[h_t, m_t], writes=[y_t])
                C.op("pool", lambda c=c: nc.gpsimd.tensor_tensor(out=y_[:, c, :], in0=y_[:, c, :], in1=r_[:, :], op=ALU.mult),
                     reads=[y_t, r_t], writes=[y_t])
                C.op("dve", lambda c=c: nc.vector.tensor_scalar(out=y_[:, c, :], in0=y_[:, c, :], scalar1=vec[:, c, 1:2], scalar2=vec[:, c, 2:3],
                                                                op0=ALU.mult, op1=ALU.add), reads=[y_t, ctok], writes=[y_t])
                C.op("act", lambda c=c: nc.scalar.activation(out=s_[:, c, :], in_=y_[:, c, :], func=AF.Silu), reads=[y_t], writes=[s_t])
            for co in range(2):
                p, pt = po.next()
                for c in range(2):
                    C.op("pe", lambda c=c, co=co: nc.tensor.matmul(p[:, :], lhsT=Wp[:, c, co * 128:(co + 1) * 128], rhs=s_[:, c, :],
                                                                  start=(c == 0), stop=(c == 1)), reads=[s_t, ctok], writes=[pt])
                o_, o_t = ob.next()
                C.op("act", lambda: _act_copy(nc, out=o_[:, :], in_=p[:, :]), reads=[pt], writes=[o_t])
                C.dma("pool", OUT[co * 128:(co + 1) * 128, t0:t0 + CH], o_[:, :], reads=[o_t])
        C.barrier()


def emit_hgrn(C, nc, S, layer, FMS, TMS, HFT, lbl_col, lbl_row, ng_col, cst, OUT):
    NT = S // 128
    with ExitStack() as es:
        def sb(name, shape, dt):
            return es.enter_context(nc.sbuf_tensor(name, shape, dt))

        def ps(name, shape, dt):
            return es.enter_context(nc.psum_tensor(name, shape, dt))
        ctok = Tok()
        Tri = sb("h_Tri", [128, 128], F32); BD = sb("h_BD", [128, 128], F32); mcol = sb("h_mcol", [128, 4], F32)
        hmask = sb("h_hm", [128, 2, 128], F32); lc = sb("h_lc", [128, 2], F32); lr = sb("h_lr", [128, 2, 128], F32)
        ngc = sb("h_ng", [128, 1], F32)
        C.dma("sp", Tri[:, :], cst["Tri"], writes=[ctok]); C.dma("sp", BD[:, :], cst["BD"], writes=[ctok])
        C.dma("sp", mcol[:, :], cst["mcol"], writes=[ctok])
        C.dma("sp", hmask[:, :, :], cst["hmask"].rearrange("h p c -> p h c"), writes=[ctok])
        C.dma("sp", lc[:, :], lbl_col, writes=[ctok]); C.dma("sp", lr[:, :, :], lbl_row, writes=[ctok])
        C.dma("sp", ngc[:, :], ng_col, writes=[ctok])
        C.barrier()
        lbc = sb("h_lbc", [128, 2], F32)
        lbr = sb("h_lbr", [128, 2, 128], F32)
        if layer == 0:
            C.op("dve", lambda: nc.vector.memset(lbc[:, 0:1], 0.0), writes=[ctok])
            C.op("dve", lambda: nc.vector.memset(lbc[:, 1:2], 1.0), writes=[ctok])
            C.op("dve", lambda: nc.vector.memset(lbr[:, 0, :], 0.0), writes=[ctok])
            C.op("dve", lambda: nc.vector.memset(lbr[:, 1, :], 1.0), writes=[ctok])
        else:
            C.op("dve", lambda: nc.vector.tensor_tensor(out=lbc[:, 0:1], in0=lc[:, 1:2], in1=lc[:, 0:1], op=ALU.subtract), writes=[ctok])
            C.op("act", lambda: nc.scalar.activation(out=lbc[:, 0:1], in_=lbc[:, 0:1], func=AF.Sigmoid), reads=[ctok], writes=[ctok])
            C.op("dve", lambda: nc.vector.tensor_scalar(out=lbc[:, 1:2], in0=lbc[:, 0:1], scalar1=-1.0, scalar2=1.0, op0=ALU.mult, op1=ALU.add),
                 reads=[ctok], writes=[ctok])
            C.op("dve", lambda: nc.vector.tensor_tensor(out=lbr[:, 0, :], in0=lr[:, 1, :], in1=lr[:, 0, :], op=ALU.subtract), reads=[ctok], writes=[ctok])
            C.op("act", lambda: nc.scalar.activation(out=lbr[:, 0, :], in_=lbr[:, 0, :], func=AF.Sigmoid), reads=[ctok], writes=[ctok])
            C.op("dve", lambda: nc.vector.tensor_scalar(out=lbr[:, 1, :], in0=lbr[:, 0, :], scalar1=-1.0, scalar2=1.0, op0=ALU.mult, op1=ALU.add),
                 reads=[ctok], writes=[ctok])
        C.barrier()
        St = sb("h_S", [128, 128], F32); Stok = Tok()
        C.op("dve", lambda: nc.vector.memset(St[:, :], 0.0), writes=[Stok])

        def R(name, shape, dt, n=2):
            return Rot([sb("h_%s%d" % (name, i), shape, dt) for i in range(n)])
        hft = R("hft", [128, 128], F32); fT = R("fT", [128, 128], F32); lfT = R("lfT", [128, 128], F32)
        e1 = R("e1", [128, 128], F32); kbT = R("kbT", [128, 128], BF16)
        hf = R("hf", [128, 128], BF16); fF = R("fF", [128, 128], F32); ebF = R("ebF", [128, 128], F32); enF = R("enF", [128, 128], F32)
        hq = R("hq", [128, 128], BF16); hg = R("hg", [128, 128], BF16); qbF = R("qbF", [128, 128], BF16); kbF = R("kbF", [128, 128], BF16)
        AT = R("AT", [128, 2, 128], BF16); iT = R("iT", [128, 128], BF16); iTm = R("iTm", [128, 4, 128], BF16); iTp = R("iTp", [128, 2, 128], BF16)
        Ub = R("Ub", [128, 4, 128], F32); Sbf = R("Sbf", [128, 4, 128], BF16)
        osb = R("osb", [128, 128], F32); osq = R("osq", [128, 128], F32); rv = R("rv", [128, 128], F32); ob = R("ob", [128, 128], BF16)
        pcT = ps("h_pcT", [128, 512], F32)[:, 0:128]; pcF = ps("h_pcF", [128, 512], F32)[:, 0:128]
        pA0 = ps("h_pA0", [128, 512], F32); pA1 = ps("h_pA1", [128, 512], F32)
        pAh = [pA0[:, 0:128], pA1[:, 0:128]]
        pU = ps("h_pU", [128, 4, 128], F32); pO = ps("h_pO", [128, 512], F32)[:, 0:128]; pQ = ps("h_pQ", [128, 512], F32)[:, 0:128]
        pcTt = Tok(); pcFt = Tok(); pAt = Tok(); pUt = Tok(); pOt = Tok(); pQt = Tok()
        for ti in range(NT):
            t0 = ti * 128
            x_, x_t = hft.next()
            C.dma("sp", x_[:, :], HFT[t0:t0 + 128, :], writes=[x_t])
            f_, f_t = fT.next()
            C.op("act", lambda: nc.scalar.activation(out=f_[:, :], in_=x_[:, :], func=AF.Sigmoid), reads=[x_t], writes=[f_t])
            C.op("dve", lambda: nc.vector.tensor_tensor(out=f_[:, :], in0=f_[:, :], in1=lbr[:, 1, :], op=ALU.mult), reads=[f_t, ctok], writes=[f_t])
            C.op("dve", lambda: nc.vector.tensor_tensor(out=f_[:, :], in0=f_[:, :], in1=lbr[:, 0, :], op=ALU.add), reads=[f_t, ctok], writes=[f_t])
            l_, l_t = lfT.next()
            C.op("act", lambda: nc.scalar.activation(out=l_[:, :], in_=f_[:, :], func=AF.Ln), reads=[f_t], writes=[l_t])
            import os
            HP = os.environ.get("HPROBE", "")
            if HP == "1":
                continue
            C.op("pe", lambda: nc.tensor.matmul(pcT[:, :], lhsT=Tri[:, :], rhs=l_[:, :], start=True, stop=True), reads=[l_t, ctok], writes=[pcTt])
            C.op("pe", lambda: nc.tensor.matmul(pcF[:, :], lhsT=l_[:, :], rhs=Tri[:, :], start=True, stop=True), reads=[l_t, ctok], writes=[pcFt])
            e_, e_t = e1.next()
            C.op("act", lambda: nc.scalar.activation(out=e_[:, :], in_=pcT[:, :], func=AF.Exp, scale=-1.0), reads=[pcTt], writes=[e_t])
            C.op("dve", lambda: nc.vector.tensor_scalar(out=f_[:, :], in0=f_[:, :], scalar1=-1.0, scalar2=1.0, op0=ALU.mult, op1=ALU.add),
                 reads=[f_t, l_t], writes=[f_t])
            kT_, kT_t = kbT.next()
            C.op("dve", lambda: nc.vector.tensor_tensor(out=kT_[:, :], in0=f_[:, :], in1=e_[:, :], op=ALU.mult), reads=[f_t, e_t], writes=[kT_t])
            if HP == "2":
                continue
            h_, h_t = hf.next(); q_, q_t = hq.next(); g_, g_t = hg.next()
            C.dma("sp", h_[:, :], FMS[FM_OFF["HF"]:FM_OFF["HF"] + 128, t0:t0 + 128], writes=[h_t])
            C.dma("sp", q_[:, :], FMS[FM_OFF["HQ"]:FM_OFF["HQ"] + 128, t0:t0 + 128], writes=[q_t])
            C.dma("sp", g_[:, :], FMS[FM_OFF["HG"]:FM_OFF["HG"] + 128, t0:t0 + 128], writes=[g_t])
            ff_, ff_t = fF.next()
            C.op("act", lambda: nc.scalar.activation(out=ff_[:, :], in_=h_[:, :], func=AF.Sigmoid), reads=[h_t], writes=[ff_t])
            C.op("dve", lambda: nc.vector.tensor_scalar(out=ff_[:, :], in0=ff_[:, :], scalar1=-1.0, scalar2=1.0, op0=ALU.mult, op1=ALU.add),
                 reads=[ff_t], writes=[ff_t])
            C.op("dve", lambda: nc.vector.tensor_scalar(out=ff_[:, :], in0=ff_[:, :], scalar1=lbc[:, 1:2], scalar2=None, op0=ALU.mult),
                 reads=[ff_t, ctok], writes=[ff_t])
            if HP == "2a":
                continue
            eb_, eb_t = ebF.next(); en_, en_t = enF.next()
            C.op("act", lambda: nc.scalar.activation(out=eb_[:, :], in_=pcF[:, :], func=AF.Exp), reads=[pcFt], writes=[eb_t])
            C.op("act", lambda: nc.scalar.activation(out=en_[:, :], in_=pcF[:, :], func=AF.Exp, scale=-1.0), reads=[pcFt], writes=[en_t])
            if HP == "2b":
                continue
            qb_, qb_t = qbF.next(); kb_, kb_t = kbF.next()
            C.op("dve", lambda: nc.vector.tensor_tensor(out=qb_[:, :], in0=q_[:, :], in1=eb_[:, :], op=ALU.mult), reads=[q_t, eb_t], writes=[qb_t])
            C.op("pool", lambda: nc.gpsimd.tensor_tensor(out=kb_[:, :], in0=ff_[:, :], in1=en_[:, :], op=ALU.mult), reads=[ff_t, en_t], writes=[kb_t])
            if HP == "2c":
                continue
            for h in range(2):
                C.op("pe", lambda h=h: nc.tensor.matmul(pAh[h], lhsT=kb_[h * 64:(h + 1) * 64, :], rhs=qb_[h * 64:(h + 1) * 64, :],
                                                       start=True, stop=True), reads=[kb_t, qb_t], writes=[pAt])
            if HP == "2d":
                continue
            A_, A_t = AT.next()
            for h in range(2):
                C.op("dve", lambda h=h: nc.vector.tensor_tensor(out=A_[:, h, :], in0=pAh[h], in1=Tri[:, :], op=ALU.mult),
                     reads=[pAt, ctok], writes=[A_t])
            if HP == "3":
                continue
            i_, i_t = iT.next()
            C.dma("sp", i_[:, :], TMS[t0:t0 + 128, 256:384], writes=[i_t])
            im_, im_t = iTm.next(); ip_, ip_t = iTp.next()
            for j in range(4):
                C.op("dve", lambda j=j: nc.vector.tensor_scalar(out=im_[:, j, :], in0=i_[:, :], scalar1=mcol[:, j:j + 1], scalar2=None, op0=ALU.mult),
                     reads=[i_t, ctok], writes=[im_t])
            for h in range(2):
                C.op("pool", lambda h=h: nc.gpsimd.tensor_tensor(out=ip_[:, h, :], in0=i_[:, :], in1=hmask[:, h, :], op=ALU.mult),
                     reads=[i_t, ctok], writes=[ip_t])
            for j in range(4):
                C.op("pe", lambda j=j: nc.tensor.matmul(pU[:, j, :], lhsT=kT_[:, :], rhs=im_[:, j, :], start=True, stop=True),
                     reads=[kT_t, im_t], writes=[pUt])
            u_, u_t = Ub.next()
            for j in range(4):
                C.op("dve", lambda j=j: nc.vector.scalar_tensor_tensor(out=u_[:, j, :], in0=pU[:, j, :], scalar=eb_[:, 32 * j + 31:32 * j + 32],
                                                                      in1=BD[:, :], op0=ALU.mult, op1=ALU.mult),
                     reads=[pUt, eb_t, ctok], writes=[u_t])
            if HP == "4":
                continue
            s_, s_t = Sbf.next()
            for j in range(4):
                C.op("act", lambda j=j: _act_copy(nc, out=s_[:, j, :], in_=St[:, :]), reads=[Stok], writes=[s_t])
                C.op("dve", lambda j=j: nc.vector.scalar_tensor_tensor(out=St[:, :], in0=St[:, :], scalar=eb_[:, 32 * j + 31:32 * j + 32],
                                                                      in1=u_[:, j, :], op0=ALU.mult, op1=ALU.add),
                     reads=[Stok, eb_t, u_t], writes=[Stok])
            if HP == "5":
                continue
            for h in range(2):
                C.op("pe", lambda h=h: nc.tensor.matmul(pO[:, :], lhsT=ip_[:, h, :], rhs=A_[:, h, :], start=(h == 0), stop=(h == 1)),
                     reads=[ip_t, A_t], writes=[pOt])
            for j in range(4):
                C.op("pe", lambda j=j: nc.tensor.matmul(pcT[:, 32 * j:32 * j + 32], lhsT=s_[:, j, :], rhs=qb_[:, 32 * j:32 * j + 32],
                                                       start=True, stop=True), reads=[s_t, qb_t, e_t, kT_t], writes=[pcTt])
            o_, o_t = osb.next(); q2, q2t = osq.next()
            C.op("act", lambda: nc.scalar.activation(out=o_[:, :], in_=pO[:, :], func=AF.Copy), reads=[pOt], writes=[o_t])
            C.op("dve", lambda: nc.vector.tensor_tensor(out=o_[:, :], in0=o_[:, :], in1=pcT[:, :], op=ALU.add), reads=[o_t, pcTt], writes=[o_t])
            if HP == "6":
                continue
            C.op("pool", lambda: nc.gpsimd.tensor_tensor(out=q2[:, :], in0=o_[:, :], in1=o_[:, :], op=ALU.mult), reads=[o_t], writes=[q2t])
            C.op("pe", lambda: nc.tensor.matmul(pQ[:, :], lhsT=BD[:, :], rhs=q2[:, :], start=True, stop=True), reads=[q2t, ctok], writes=[pQt])
            r_, r_t = rv.next()
            C.op("dve", lambda: nc.vector.tensor_scalar(out=r_[:, :], in0=pQ[:, :], scalar1=1.0 / 64.0, scalar2=RMS_EPS, op0=ALU.mult, op1=ALU.add),
                 reads=[pQt], writes=[r_t])
            C.op("act", lambda: nc.scalar.activation(out=r_[:, :], in_=r_[:, :], func=AF.Sqrt), reads=[r_t], writes=[r_t])
            C.op("dve", lambda: nc.vector.reciprocal(out=r_[:, :], in_=r_[:, :]), reads=[r_t], writes=[r_t])
            C.op("dve", lambda: nc.vector.tensor_tensor(out=o_[:, :], in0=o_[:, :], in1=r_[:, :], op=ALU.mult), reads=[o_t, r_t], writes=[o_t])
            b_, b_t = ob.next()
            C.op("dve", lambda: nc.vector.scalar_tensor_tensor(out=b_[:, :], in0=o_[:, :], scalar=ngc[:, 0:1], in1=g_[:, :], op0=ALU.mult, op1=ALU.mult),
                 reads=[o_t, g_t, ctok], writes=[b_t])
            C.dma("pool", OUT[:, t0:t0 + 128], b_[:, :], reads=[b_t])
        C.barrier()


def _rel_bucket(dist):
    n = np.maximum(dist, 0)
    nf = np.maximum(n, 1).astype(np.float32)
    large = 16 + (np.log(nf / np.float32(16)) / np.float32(math.log(128 / 16)) * np.float32(16)).astype(np.int32)
    return np.where(n < 16, n, np.minimum(large, 31)).astype(np.int64)


def _bias_tiles(tab, dist, valid):
    b = _rel_bucket(dist)
    out = tab[:, b]
    return np.where(valid[None], out, np.float32(NEG)).astype(np.float32)


def host_consts(S):
    c = {}
    c["ident"] = np.eye(128, dtype=np.float32).astype(NPBF)
    c["ones64"] = np.ones((128, 64), np.float32)
    m = np.ones((128, 1), np.float32); m[127] = 0
    c["m127"] = m
    s = np.arange(128)
    c["Tri"] = ((s[:, None] // 32 == s[None, :] // 32) & (s[:, None] <= s[None, :])).astype(np.float32)
    c["BD"] = (s[:, None] // 64 == s[None, :] // 64).astype(np.float32)
    c["mcol"] = (s[:, None] // 32 == np.arange(4)[None, :]).astype(np.float32)
    c["hmask"] = np.stack([np.broadcast_to((s // 64 == h)[None, :], (128, 128)) for h in range(2)]).astype(np.float32)
    NSEL = S // 64; NB = S // 256; NCMP = (S - 32) // 16 + 1; NTC = (NCMP + 127) // 128
    n = np.arange(NTC * 128); j = np.arange(NSEL)
    cs = n * 16; ce = cs + 31; ss = j * 64
    ov = np.clip(np.minimum(ce[:, None] + 1, ss[None, :] + 64) - np.maximum(cs[:, None], ss[None, :]), 0, None).astype(np.float32) / 32.0
    ov[:, NSEL - 1] = 1.0
    ov[NCMP:, :] = 0.0
    c["OV"] = ov
    t = np.arange(S); blk = t // 64
    forced = (j[None, :] == 0) | (j[None, :] == blk[:, None]) | (j[None, :] == blk[:, None] - 1)
    valid = j[None, :] <= blk[:, None]
    st = np.where(forced, 2.0e9, np.where(valid, 0.0, -2.0e9)).astype(np.float32)
    c["STAT"] = st.reshape(S // 128, 128, NSEL)
    c["Gd"] = (np.arange(S)[None, :] // 64 == j[:, None]).astype(np.float32).astype(NPBF)
    jb = np.arange(NB); own = t // 256
    stm = np.where(jb[None, :] > own[:, None], -2.0e9, np.where(jb[None, :] == own[:, None], 2.0e9, 0.0)).astype(np.float32)
    c["STATM"] = stm.reshape(S // 128, 128, NB)
    c["G2d"] = (np.arange(S)[None, :] // 256 == (np.arange(128) % NB)[:, None]).astype(np.float32).astype(NPBF)
    return c


def host_bias(rel_bias, p):
    nsa_heads = [2 * p, 2 * p + 1, 2 * (1 - p), 2 * (1 - p) + 1]
    tabn = rel_bias[:4][nsa_heads]
    tabm = rel_bias[4:][[2 * p, 2 * p + 1]]
    ki = np.arange(128)[:, None]; qi = np.arange(512)[None, :]
    btc = []
    for pi in range(5):
        dist = 512 * pi + qi - 16 * ki - 31
        btc.append(_bias_tiles(tabn, dist, dist >= 0))
    BTc = np.stack(btc, 1).reshape(20, 128, 512)
    bts = []; btm = []
    for pi in range(5):
        dist = (128 * pi - 384) + qi - ki
        bts.append(_bias_tiles(tabn[:2], dist, dist >= 0))
        btm.append(_bias_tiles(tabm, dist, dist >= 0))
    BTs = np.stack(bts, 1).reshape(10, 128, 512)
    BTm = np.stack(btm, 1).reshape(10, 128, 512)
    btw = []
    for pi in range(8):
        dist = (128 * pi - 384) + qi - ki
        btw.append(_bias_tiles(tabn[:2], dist, (dist >= 0) & (dist < 512)))
    BTw = np.stack(btw, 1).reshape(16, 128, 512)
    t31 = np.ascontiguousarray(np.broadcast_to(tabn[:, 31][None, :], (128, 4))).astype(np.float32)
    t31m = np.ascontiguousarray(np.broadcast_to(tabm[:, 31][None, :], (128, 2))).astype(np.float32)
    return dict(BTc=BTc, BTs=BTs, BTw=BTw, BTm=BTm, t31=t31, t31m=t31m)


def host_wsplit(w_in_l, p):
    def cols(a, n):
        return list(range(a, a + n))
    fm = (cols(128 * p, 128) + cols(128 * (1 - p), 128) + cols(256, 128) + cols(384, 64) + cols(512, 64) + cols(640 + 6 * p, 6)
          + cols(652 + 128 * p, 128) + cols(908 + 128 * p, 128) + cols(1420, 512)
          + cols(1932 + 128 * p, 128) + cols(2188 + 128 * p, 128) + cols(2700 + 128 * p, 128))
    tm = cols(448, 64) + cols(576, 64) + cols(1164 + 128 * p, 128) + cols(2444 + 128 * p, 128) + cols(2188 + 128 * p, 128)
    assert len(fm) == NFM_COLS and len(tm) == NTM_COLS
    return np.ascontiguousarray(w_in_l[:, fm]), np.ascontiguousarray(w_in_l[:, tm])


MIX_CONST_NAMES = ["ident", "ones64", "m127", "Tri", "BD", "mcol", "hmask", "OV", "STAT", "Gd", "STATM", "G2d"]
MIX_BIAS_NAMES = ["BTc", "BTs", "BTw", "BTm", "t31", "t31m"]


def build_mix(S, layer, x_f32, parts=("nsa", "moba", "conv", "hgrn")):
    nc = bass.Bass("TRN2", target_bir_lowering=False)
    hc = host_consts(S)
    hb = host_bias(np.zeros((8, 32), np.float32), 0)

    def din(name, arr_or_shape, dt=None):
        if isinstance(arr_or_shape, np.ndarray):
            shape = list(arr_or_shape.shape)
            dt = BF16 if arr_or_shape.dtype == NPBF else F32
        else:
            shape = list(arr_or_shape)
        return nc.dram_tensor(name, shape, dt, kind="ExternalInput").ap()
    d = {}
    need = set()
    if "nsa" in parts:
        need |= {"ident", "ones64", "m127", "OV", "STAT", "Gd", "BTc", "BTs", "BTw", "t31"}
    if "moba" in parts:
        need |= {"ident", "ones64", "STATM", "G2d", "BTm", "t31m"}
    if "conv" in parts:
        need |= {"ident"}
    if "hgrn" in parts:
        need |= {"Tri", "BD", "mcol", "hmask"}
    for k in MIX_CONST_NAMES:
        if k in need:
            d[k] = din(k, hc[k])
    for k in MIX_BIAS_NAMES:
        if k in need:
            d[k] = din(k, hb[k])
    xT = din("xT", [1024, S], F32 if x_f32 else BF16)
    wfm = din("wfm", [1024, NFM_COLS], F32); wtm = din("wtm", [1024, NTM_COLS], F32)
    if "nsa" in parts:
        cw1 = din("cw1", [2, 2048, 256], F32); cb1 = din("cb1", [2, 256], F32); cw2 = din("cw2", [2, 256, 64], F32); cpe = din("cpe", [2, 32, 64], F32)
    if "conv" in parts:
        dww = din("dww", [31, 256], F32); dwb = din("dwb", [256], F32); clg = din("clg", [256], F32); clb = din("clb", [256], F32)
        pww = din("pww", [256, 256], F32)
    if "hgrn" in parts:
        lbl_col = din("lbl_col", [128, 2], F32); lbl_row = din("lbl_row", [128, 2, 128], F32); ng_col = din("ng_col", [128, 1], F32)
    FMS = nc.dram_tensor("FMS", [NFM_ROWS, S], BF16, kind="Internal" if parts else "ExternalOutput").ap()
    TMS = nc.dram_tensor("TMS", [S, 384], BF16, kind="Internal" if parts else "ExternalOutput").ap()
    HFT = nc.dram_tensor("HFT", [S, 128], F32, kind="Internal" if parts else "ExternalOutput").ap()
    if "nsa" in parts:
        ONSA = nc.dram_tensor("ONSA", [128, S], BF16, kind="ExternalOutput").ap()
    if "moba" in parts:
        OMOBA = nc.dram_tensor("OMOBA", [128, S], BF16, kind="ExternalOutput").ap()
    if "conv" in parts:
        OCONV = nc.dram_tensor("OCONV", [256, S], BF16, kind="ExternalOutput").ap()
    if "hgrn" in parts:
        OHGRN = nc.dram_tensor("OHGRN", [128, S], BF16, kind="ExternalOutput").ap()
    nc._mix_inputs = set(d.keys()) | {"xT", "wfm", "wtm"} | ({"cw1", "cb1", "cw2", "cpe"} if "nsa" in parts else set()) | \
        ({"dww", "dwb", "clg", "clb", "pww"} if "conv" in parts else set()) | ({"lbl_col", "lbl_row", "ng_col"} if "hgrn" in parts else set())
    with ExitStack() as st:
        st.enter_context(nc.allow_non_contiguous_dma(reason="tiny constant / vector loads"))
        C = Ctx(nc, st)
        emit_proj(C, nc, S, xT, x_f32, wfm, wtm, FMS, TMS, HFT)
        if "nsa" in parts:
            emit_nsa(C, nc, S, FMS, TMS, cw1, cb1, cw2, cpe, d["BTc"], d["BTs"], d["BTw"], d["t31"], d["STAT"], d["OV"], d["Gd"], d, ONSA)
        if "moba" in parts:
            emit_moba(C, nc, S, FMS, TMS, d["BTm"], d["t31m"], d["STATM"], d["G2d"], d, OMOBA)
        if "conv" in parts:
            emit_conv(C, nc, S, FMS, dww, dwb, clg, clb, pww, d, OCONV)
        if "hgrn" in parts:
            emit_hgrn(C, nc, S, layer, FMS, TMS, HFT, lbl_col, lbl_row, ng_col, d, OHGRN)
        C.finish()
    return nc, hc


def mix_inputs(hc, p, l, xT, inp):
    m = {k: hc[k] for k in MIX_CONST_NAMES}
    m.update(host_bias(np.asarray(inp["rel_bias"], np.float32), p))
    wfm, wtm = host_wsplit(np.asarray(inp["w_in"][l]), p)
    m["xT"] = xT; m["wfm"] = wfm; m["wtm"] = wtm
    m["cw1"] = np.asarray(inp["nsa_cmp_w1"][l]); m["cb1"] = np.asarray(inp["nsa_cmp_b1"][l])
    m["cw2"] = np.asarray(inp["nsa_cmp_w2"][l]); m["cpe"] = np.asarray(inp["nsa_cmp_pe"][l])
    m["dww"] = np.asarray(inp["conv_dw_w"][l]); m["dwb"] = np.asarray(inp["conv_dw_b"][l])
    m["clg"] = np.asarray(inp["conv_ln_g"][l]); m["clb"] = np.asarray(inp["conv_ln_b"][l]); m["pww"] = np.asarray(inp["conv_pw_w"][l])
    lb = np.asarray(inp["hgrn_lb_logits"])[:, 128 * p:128 * (p + 1)]
    m["lbl_col"] = np.ascontiguousarray(lb.T)
    m["lbl_row"] = np.ascontiguousarray(np.broadcast_to(lb[None], (128, 2, 128)))
    m["ng_col"] = np.ascontiguousarray(np.asarray(inp["hgrn_norm_g"][l])[128 * p:128 * (p + 1), None])
    return {k: np.ascontiguousarray(v) for k, v in m.items()}


def build_tail(ntok, last):
    nc = bass.Bass("TRN2", target_bir_lowering=False)

    def din(name, shape, dt):
        return nc.dram_tensor(name, shape, dt, kind="ExternalInput").ap()
    xres = din("xres", [ntok, 1024], F32); catT = din("catT", [1024, ntok], BF16)
    wo = din("w_out", [1024, 1024], F32); w1 = din("w_ff1", [1024, 4096], F32); w2 = din("w_ff2", [4096, 1024], F32)
    lnc = din("lnc", [4, 128, 1024], F32); idd = din("ident", [128, 128], BF16)
    x1d = nc.dram_tensor("x1d", [ntok, 1024], F32, kind="Internal").ap()
    x1Td = nc.dram_tensor("x1Td", [1024, ntok], BF16, kind="Internal").ap()
    xn = nc.dram_tensor("xn", [ntok, 1024], F32, kind="ExternalOutput").ap()
    xnT = None if last else nc.dram_tensor("xnT", [1024, ntok], BF16, kind="ExternalOutput").ap()
    with ExitStack() as st:
        C = Ctx(nc, st)
        emit_tail_a(C, nc, ntok, xres, catT, wo, lnc, x1d, x1Td, idd)
        emit_tail_b(C, nc, ntok, x1d, x1Td, w1, w2, lnc, xn, xnT, idd)
        C.finish()
    return nc


def kernel(**inp):
    inp = {k: np.asarray(v) for k, v in inp.items()}
    x = inp["x"].astype(np.float32)
    B, S, D = x.shape
    ncores = 8
    half = S // 2
    ident = np.eye(128, dtype=np.float32).astype(NPBF)
    xres = [np.ascontiguousarray(x[c // 2, (c % 2) * half:(c % 2 + 1) * half]) for c in range(ncores)]
    xT = [np.ascontiguousarray(x[c // 2].T) for c in range(ncores)]
    for l in range(DEPTH):
        nc, hc = build_mix(S, l, l == 0)
        maps = []
        for c in range(ncores):
            m = mix_inputs(hc, c % 2, l, xT[c], inp)
            maps.append({k: v for k, v in m.items() if k in nc._mix_inputs})
        res = run_bass_kernel_spmd(nc, maps, core_ids=list(range(ncores))).results
        cats = []
        for b in range(B):
            r0, r1 = res[2 * b], res[2 * b + 1]
            cats.append(np.concatenate([np.asarray(r0["ONSA"]), np.asarray(r1["ONSA"]), np.asarray(r0["OMOBA"]), np.asarray(r1["OMOBA"]),
                                        np.asarray(r0["OCONV"]), np.asarray(r0["OHGRN"]), np.asarray(r1["OHGRN"])], axis=0))
        nct = build_tail(half, l == DEPTH - 1)
        lnc = np.stack([np.broadcast_to(inp[k][l], (128, D)) for k in ("ln1_g", "ln1_b", "ln2_g", "ln2_b")]).astype(np.float32)
        maps = []
        for c in range(ncores):
            b, p = c // 2, c % 2
            maps.append({"xres": xres[c], "catT": np.ascontiguousarray(cats[b][:, p * half:(p + 1) * half]),
                         "w_out": np.ascontiguousarray(inp["w_out"][l]), "w_ff1": np.ascontiguousarray(inp["w_ff1"][l]),
                         "w_ff2": np.ascontiguousarray(inp["w_ff2"][l]), "lnc": np.ascontiguousarray(lnc), "ident": ident})
        res = run_bass_kernel_spmd(nct, maps, core_ids=list(range(ncores))).results
        xres = [np.asarray(res[c]["xn"]) for c in range(ncores)]
        if l < DEPTH - 1:
            xT = []
            for c in range(ncores):
                b = c // 2
                xT.append(np.ascontiguousarray(np.concatenate([np.asarray(res[2 * b]["xnT"]), np.asarray(res[2 * b + 1]["xnT"])], axis=1)))
    out = np.stack([np.concatenate([xres[2 * b], xres[2 * b + 1]], axis=0) for b in range(B)]).astype(np.float32)
    return out
```
